# Optimizing a Trainium2 kernel written in Bass

```python
import math
import jax, jax.numpy as jnp
from jax import lax
import numpy as np

D_MODEL = 1024
BATCH = 16
SEQ = 2048
DEPTH = 1

N_ATTN_HEADS = 8
HEAD_DIM = 64
D_ATTN = N_ATTN_HEADS * HEAD_DIM
MOBA_BLOCK = 256
MOBA_TOPK = 3
Q_CHUNK = 16
N_BUCKETS = 32
MAX_EXACT = N_BUCKETS // 2
MAX_DISTANCE = 128
D_LRU = D_MODEL // 2
N_LRU_BLOCKS = 8
LRU_BLOCK = D_LRU // N_LRU_BLOCKS
LRU_CONV_W = 4
LRU_C = 8.0
D_MIX = D_ATTN + D_LRU
D_IN_PROJ = 3 * D_ATTN + 2 * D_LRU
D_FF = 2816
FFN_CONV_W = 3
RMS_EPS = 1e-6

kernel_name = "hymba_moba_rglru_convffn_sandwich"


def rms_norm(x, g):
    xf = x.astype(jnp.float32)
    y = xf * lax.rsqrt(jnp.mean(xf * xf, axis=-1, keepdims=True) + RMS_EPS)
    return (y * g.astype(jnp.float32)).astype(x.dtype)


def causal_dwconv(x, w, b):
    width, ch = w.shape
    y = lax.conv_general_dilated(
        x, w[:, None, :].astype(x.dtype), window_strides=(1,), padding=[(width - 1, 0)],
        dimension_numbers=("NWC", "WIO", "NWC"), feature_group_count=ch)
    return y + b.astype(x.dtype)


def t5_bucket(dist):
    n = jnp.maximum(dist, 0)
    nf = jnp.maximum(n, 1).astype(jnp.float32)
    large = MAX_EXACT + (jnp.log(nf / MAX_EXACT) / math.log(MAX_DISTANCE / MAX_EXACT)
                         * (N_BUCKETS - MAX_EXACT)).astype(jnp.int32)
    large = jnp.minimum(large, N_BUCKETS - 1)
    return jnp.where(n < MAX_EXACT, n, large)


def moba_attention(q, k, v, rel_bias):
    B, H, S, dh = q.shape
    nb = -(-S // MOBA_BLOCK)
    pad = nb * MOBA_BLOCK - S
    k_pad = jnp.pad(k, ((0, 0), (0, 0), (0, pad), (0, 0)))
    v_pad = jnp.pad(v, ((0, 0), (0, 0), (0, pad), (0, 0)))
    k_blocks = k_pad.reshape(B, H, nb, MOBA_BLOCK, dh)
    v_blocks = v_pad.reshape(B, H, nb, MOBA_BLOCK, dh)
    k_mean = jnp.mean(k_blocks.astype(jnp.float32), axis=3)
    top = min(MOBA_TOPK, nb)
    scale = HEAD_DIM ** -0.5
    key_off = jnp.arange(MOBA_BLOCK, dtype=jnp.int32)
    b_i = jnp.arange(B)[:, None, None, None]
    h_i = jnp.arange(H)[None, :, None, None]
    bias_T = rel_bias.astype(jnp.float32).T

    def chunk(c):
        q0 = c * Q_CHUNK
        qc = lax.dynamic_slice_in_dim(q, q0, Q_CHUNK, axis=2)
        q_pos = q0 + jnp.arange(Q_CHUNK, dtype=jnp.int32)
        own = q0 // MOBA_BLOCK
        gate = jnp.einsum("bhqd,bhnd->bhqn", qc.astype(jnp.float32), k_mean)
        past = jnp.arange(nb) < own
        gate = jnp.where(past[None, None, None, :], gate, -jnp.inf)
        _, idx = lax.top_k(gate, top)
        valid = idx < own
        k_sel = k_blocks[b_i, h_i, idx]
        v_sel = v_blocks[b_i, h_i, idx]
        s_sel = jnp.einsum("bhqd,bhqjkd->bhqjk", qc, k_sel,
                           preferred_element_type=jnp.float32) * scale
        k_pos_sel = idx[..., None] * MOBA_BLOCK + key_off
        bucket_sel = t5_bucket(q_pos[None, None, :, None, None] - k_pos_sel)
        s_sel = s_sel + bias_T[h_i[..., None], bucket_sel]
        s_sel = jnp.where(valid[..., None], s_sel, -jnp.inf)
        k_own = lax.dynamic_slice_in_dim(k_pad, own * MOBA_BLOCK, MOBA_BLOCK, axis=2)
        v_own = lax.dynamic_slice_in_dim(v_pad, own * MOBA_BLOCK, MOBA_BLOCK, axis=2)
        s_own = jnp.einsum("bhqd,bhkd->bhqk", qc, k_own,
                           preferred_element_type=jnp.float32) * scale
        dist_own = q_pos[:, None] - (own * MOBA_BLOCK + key_off)[None, :]
        s_own = s_own + jnp.transpose(rel_bias.astype(jnp.float32)[t5_bucket(dist_own)], (2, 0, 1))[None]
        s_own = jnp.where((dist_own >= 0)[None, None], s_own, -jnp.inf)
        logits = jnp.concatenate([s_sel.reshape(B, H, Q_CHUNK, top * MOBA_BLOCK), s_own], axis=-1)
        p = jax.nn.softmax(logits, axis=-1).astype(v.dtype)
        p_sel = p[..., :top * MOBA_BLOCK].reshape(B, H, Q_CHUNK, top, MOBA_BLOCK)
        p_own = p[..., top * MOBA_BLOCK:]
        return (jnp.einsum("bhqjk,bhqjkd->bhqd", p_sel, v_sel)
                + jnp.einsum("bhqk,bhkd->bhqd", p_own, v_own))

    outs = lax.map(chunk, jnp.arange(S // Q_CHUNK))
    return jnp.transpose(outs, (1, 2, 0, 3, 4)).reshape(B, H, S, dh)


def rg_lru(x, w_r, b_r, w_i, b_i, lam):
    B, S, _ = x.shape
    xf = x.astype(jnp.float32)
    xb = xf.reshape(B, S, N_LRU_BLOCKS, LRU_BLOCK)
    r = jax.nn.sigmoid(jnp.einsum("bsnc,ncd->bsnd", xb, w_r.astype(jnp.float32)).reshape(B, S, D_LRU)
                       + b_r.astype(jnp.float32))
    i = jax.nn.sigmoid(jnp.einsum("bsnc,ncd->bsnd", xb, w_i.astype(jnp.float32)).reshape(B, S, D_LRU)
                       + b_i.astype(jnp.float32))
    log_a = -LRU_C * r * jax.nn.softplus(-lam.astype(jnp.float32))
    a = jnp.exp(log_a)
    u = jnp.sqrt(-jnp.expm1(2.0 * log_a)) * (i * xf)

    def combine(left, right):
        a1, b1 = left
        a2, b2 = right
        return a1 * a2, a2 * b1 + b2

    _, h = lax.associative_scan(combine, (a, u), axis=1)
    return h.astype(x.dtype)


def setup_inputs(seed: int = 0) -> dict:
    key = jax.random.key(seed)
    ks = jax.random.split(key, 24)
    nrm = lambda k, shape, s: jax.random.normal(k, shape, jnp.float32) * s
    u_a = jax.random.uniform(ks[10], (DEPTH, D_LRU), jnp.float32, 0.9, 0.999)
    base = u_a ** (1.0 / LRU_C)
    lam = jnp.log(base) - jnp.log1p(-base)
    return {
        "x": nrm(ks[0], (BATCH, SEQ, D_MODEL), 1.0),
        "g_pre_mix": 1.0 + nrm(ks[1], (DEPTH, D_MODEL), 0.05),
        "w_in": nrm(ks[2], (DEPTH, D_MODEL, D_IN_PROJ), D_MODEL ** -0.5),
        "rel_bias": nrm(ks[3], (N_BUCKETS, N_ATTN_HEADS), 0.3),
        "w_conv_lru": nrm(ks[4], (DEPTH, LRU_CONV_W, D_LRU), LRU_CONV_W ** -0.5),
        "b_conv_lru": nrm(ks[5], (DEPTH, D_LRU), 0.02),
        "w_r": nrm(ks[6], (DEPTH, N_LRU_BLOCKS, LRU_BLOCK, LRU_BLOCK), LRU_BLOCK ** -0.5),
        "b_r": nrm(ks[7], (DEPTH, D_LRU), 0.02),
        "w_i": nrm(ks[8], (DEPTH, N_LRU_BLOCKS, LRU_BLOCK, LRU_BLOCK), LRU_BLOCK ** -0.5),
        "b_i": nrm(ks[9], (DEPTH, D_LRU), 0.02),
        "lam": lam,
        "w_out": nrm(ks[11], (DEPTH, D_MIX, D_MODEL), D_MIX ** -0.5),
        "g_post_mix": 1.0 + nrm(ks[12], (DEPTH, D_MODEL), 0.05),
        "g_pre_ffn": 1.0 + nrm(ks[13], (DEPTH, D_MODEL), 0.05),
        "w_up": nrm(ks[14], (DEPTH, D_MODEL, 2 * D_FF), D_MODEL ** -0.5),
        "w_conv_ffn": nrm(ks[15], (DEPTH, FFN_CONV_W, 2 * D_FF), FFN_CONV_W ** -0.5),
        "b_conv_ffn": nrm(ks[16], (DEPTH, 2 * D_FF), 0.02),
        "w_down": nrm(ks[17], (DEPTH, D_FF, D_MODEL), D_FF ** -0.5),
        "g_post_ffn": 1.0 + nrm(ks[18], (DEPTH, D_MODEL), 0.05),
    }


def reference(x, g_pre_mix, w_in, rel_bias, w_conv_lru, b_conv_lru, w_r, b_r, w_i, b_i, lam,
              w_out, g_post_mix, g_pre_ffn, w_up, w_conv_ffn, b_conv_ffn, w_down, g_post_ffn):
    B, S, _ = x.shape
    for l in range(DEPTH):
        h = rms_norm(x, g_pre_mix[l])
        proj = h @ w_in[l].astype(h.dtype)
        q, k, v, xr, gr = jnp.split(
            proj, [D_ATTN, 2 * D_ATTN, 3 * D_ATTN, 3 * D_ATTN + D_LRU], axis=-1)
        to_heads = lambda t: jnp.transpose(t.reshape(B, S, N_ATTN_HEADS, HEAD_DIM), (0, 2, 1, 3))
        attn = moba_attention(to_heads(q), to_heads(k), to_heads(v), rel_bias)
        attn = jnp.transpose(attn, (0, 2, 1, 3)).reshape(B, S, D_ATTN)
        xr = causal_dwconv(xr, w_conv_lru[l], b_conv_lru[l])
        lru = rg_lru(xr, w_r[l], b_r[l], w_i[l], b_i[l], lam[l]) * jax.nn.gelu(gr, approximate=True)
        mix = jnp.concatenate([attn, lru], axis=-1) @ w_out[l].astype(h.dtype)
        x = x + rms_norm(mix, g_post_mix[l])
        h = rms_norm(x, g_pre_ffn[l])
        up = causal_dwconv(h @ w_up[l].astype(h.dtype), w_conv_ffn[l], b_conv_ffn[l])
        c_u, c_g = jnp.split(up, 2, axis=-1)
        ffn = (jax.nn.gelu(c_g, approximate=True) * c_u) @ w_down[l].astype(h.dtype)
        x = x + rms_norm(ffn, g_post_ffn[l])
    return x
```

```python
import math
import os
from contextlib import ExitStack

import numpy as np
import concourse.bass as bass
import concourse.mybir as mybir
from concourse.bass_utils import run_bass_kernel_spmd

F32 = mybir.dt.float32
BF16 = mybir.dt.bfloat16
AF = mybir.ActivationFunctionType
ALU = mybir.AluOpType
AX = mybir.AxisListType

NCORES = 8
SEQ = 2048
D = 1024
DFF = 2816
NEG = -30000.0
EPS = 1e-6


class Buf:
    __slots__ = ("name", "w", "r")

    def __init__(self, name):
        self.name = name
        self.w = {}
        self.r = {}


class TK:
    def __init__(self, nc, st):
        self.nc = nc
        self.st = st
        self.E = {"pe": nc.tensor, "act": nc.scalar, "dve": nc.vector, "pool": nc.gpsimd, "sp": nc.sync}
        self.sem = {e: st.enter_context(nc.semaphore("s_" + e)) for e in ("pe", "act", "dve", "pool")}
        self.cnt = {e: 0 for e in self.sem}
        self.waited = {e: {} for e in self.E}
        self.dsem = {}
        self.dcnt = {}

    def _deps(self, R, W):
        d = {}
        for b in R:
            for k, sv in b.w.items():
                if k not in d or d[k][1] < sv[1]:
                    d[k] = sv
        for b in W:
            for src in (b.w, b.r):
                for k, sv in src.items():
                    if k not in d or d[k][1] < sv[1]:
                        d[k] = sv
        return d

    def _wait(self, e, d, skip=None):
        for k, (s, v) in d.items():
            if k == skip:
                continue
            if self.waited[e].get(k, 0) >= v:
                continue
            self.E[e].wait_ge(s, v)
            self.waited[e][k] = v

    def op(self, e, fn, R=(), W=(), inc=True):
        d = self._deps(R, W)
        self._wait(e, d, skip=("pe" if e == "pe" else None))
        ins = fn()
        if inc:
            self.cnt[e] += 1
            ins.then_inc(self.sem[e], 1)
            tv = self.cnt[e]
        else:
            tv = self.cnt[e] + 1
        t = (self.sem[e], tv)
        for b in W:
            b.w = {e: t}
            b.r = {}
        for b in R:
            if b not in W:
                b.r[e] = t
        return ins

    def dma(self, q, key, out, in_, R=(), W=()):
        if key not in self.dsem:
            self.dsem[key] = self.st.enter_context(self.nc.semaphore("d_" + key))
            self.dcnt[key] = 0
        d = self._deps(R, W)
        self._wait(q, d, skip=key)
        ins = self.E[q].dma_start(out=out, in_=in_)
        self.dcnt[key] += 16
        ins.then_inc(self.dsem[key], 16)
        t = (self.dsem[key], self.dcnt[key])
        for b in W:
            b.w = {key: t}
            b.r = {}
        for b in R:
            if b not in W:
                b.r[key] = t

    def barrier(self, bufs):
        for e in self.E:
            for k, s in self.sem.items():
                if self.cnt[k] > 0 and self.waited[e].get(k, 0) < self.cnt[k]:
                    self.E[e].wait_ge(s, self.cnt[k])
                    self.waited[e][k] = self.cnt[k]
            for k, s in self.dsem.items():
                if self.dcnt[k] > 0 and self.waited[e].get(k, 0) < self.dcnt[k]:
                    self.E[e].wait_ge(s, self.dcnt[k])
                    self.waited[e][k] = self.dcnt[k]
        for b in bufs:
            b.w = {}
            b.r = {}


class _Stop(Exception):
    pass


_STOP = float(os.environ.get('KSTOP', '99'))


def ck(n):
    if n >= _STOP:
        raise _Stop()


def build_nc():
    nc = bass.Bass("TRN2", target_bir_lowering=False)

    def din(n, s):
        return nc.dram_tensor(n, s, F32, kind="ExternalInput").ap()

    x_d = din("x", [2 * SEQ, D])
    y_d = nc.dram_tensor("y", [2 * SEQ, D], F32, kind="ExternalOutput").ap()
    w_in_d = din("w_in", [D, 2560])
    w_out_d = din("w_out", [D, D])
    w_up_d = din("w_up", [D, 2 * DFF])
    w_down_d = din("w_down", [DFF, D])
    gains_d = din("gains", [4, D])
    lrucols_d = din("lrucols", [128, 32])
    wrbd_d = din("wrbd", [128, 512])
    wibd_d = din("wibd", [128, 512])
    ffncols_d = din("ffncols", [128, 176])
    btraw_d = din("btraw", [128, 2048])
    c31_d = din("c31", [1, 8])
    khot_d = din("khot", [8, SEQ])
    ident_d = din("ident", [128, 128])
    pm_d = din("pm", [1, 512])

    w_in_v = w_in_d.rearrange("(k p) c -> p k c", p=128)
    w_out_v = w_out_d.rearrange("(k p) c -> p k c", p=128)
    w_up_v = w_up_d.rearrange("(k p) c -> p k c", p=128)
    w_down_v = w_down_d.rearrange("(j p) c -> p j c", p=128)

    with ExitStack() as st:
        T = TK(nc, st)
        allbufs = []

        def mkbuf(name):
            b = Buf(name)
            allbufs.append(b)
            return b

        def sb(name, cols, dtype):
            return nc.alloc_sbuf_tensor("sb_" + name, [128, cols], dtype)

        E_h = sb("E", 8 * SEQ, BF16)
        E_v = E_h[:, :].rearrange("p (k t) -> p k t", k=8)
        gains_h = sb("gains", 4 * D, F32)
        xt_h = sb("xt", 4 * D, F32)
        xn_h = [sb("xn%d" % i, D, BF16) for i in range(2)]
        hT_h = sb("hT", 8 * 512, BF16)
        hT_v = hT_h[:, :].rearrange("p (k t) -> p k t", k=8)
        btb_h = sb("btb", 8 * 256, BF16)
        pm_h = sb("pm", 512, F32)
        wrbd_h = sb("wrbd", 512, BF16)
        wibd_h = sb("wibd", 512, BF16)
        ident_h = sb("ident", 128, BF16)
        onesf_h = sb("onesf", 128, F32)
        lrucols_h = sb("lrucols", 32, F32)
        ffncols_h = sb("ffncols", 176, F32)
        c31_h = sb("c31", 8, F32)
        stat_h = sb("stat", 128, F32)
        junk_h = sb("junk", D, BF16)
        misc_h = sb("misc", 16, F32)
        ARENA_BYTES = 115200
        arena = sb("arena", ARENA_BYTES // 4, F32)

        class Carver:
            def __init__(self):
                self.off = 0

            def take(self, nelem, dtype):
                nbytes = nelem * (2 if dtype == BF16 else 4)
                nbytes = (nbytes + 31) // 32 * 32
                assert self.off + nbytes <= ARENA_BYTES, (self.off, nbytes)
                v = arena[:, self.off // 4:(self.off + nbytes) // 4]
                self.off += nbytes
                if dtype == BF16:
                    v = v.bitcast(BF16)
                return v[:, 0:nelem]

        cm = Carver()
        winr = [cm.take(8 * 256, BF16).rearrange("p (k c) -> p k c", k=8) for _ in range(4)]
        kT = cm.take(8 * SEQ, BF16).rearrange("p (h t) -> p h t", h=8)
        vaug = cm.take(16 * 4 * 192, BF16).rearrange("p (a b c) -> p a b c", a=16, b=4)
        qT = cm.take(8 * 512, BF16).rearrange("p (h t) -> p h t", h=8)
        pT = [cm.take(512, BF16) for _ in range(3)]
        xr = cm.take(4 * 516, F32).rearrange("p (c t) -> p c t", c=4)
        gg = cm.take(4 * 512, BF16).rearrange("p (c t) -> p c t", c=4)
        ybuf = cm.take(512, F32)
        ybf = cm.take(512, BF16)
        rb = cm.take(512, F32)
        ib = cm.take(512, F32)
        ub = cm.take(512, F32)
        hs = cm.take(512, F32)
        kmean = cm.take(64, BF16).rearrange("p (h j) -> p h j", h=8)
        Msel = cm.take(8 * 72, BF16).rearrange("p (h c) -> p h c", h=8)
        gm = cm.take(64, F32)
        top8 = cm.take(64, F32)
        thr = cm.take(8, F32)
        selb = cm.take(64, F32)
        rden = cm.take(512, F32)
        bcsb = cm.take(512, F32)
        cf = Carver()
        wdown = cf.take(22 * D, BF16).rearrange("p (j c) -> p j c", j=22)
        wout = cf.take(8 * D, BF16).rearrange("p (k c) -> p k c", k=8)
        wupr = [cf.take(8 * 256, BF16).rearrange("p (k c) -> p k c", k=8) for _ in range(4)]
        actT = cf.take(22 * 512, BF16).rearrange("p (j t) -> p j t", j=22)
        xub = [cf.take(514, F32) for _ in range(2)]
        xgb = [cf.take(514, F32) for _ in range(2)]
        cu = cf.take(512, F32)
        cg = cf.take(512, F32)
        gl = cf.take(512, F32)
        halo = cf.take(88, F32).rearrange("p (j t) -> p j t", j=44)

        ps = [nc.alloc_psum_tensor("ps%d" % i, [128, 512], F32) for i in range(8)]
        ps7b = ps[7][:, :].bitcast(BF16)

        B_ps = [mkbuf("ps%d" % i) for i in range(8)]
        B_E = [mkbuf("E%d" % i) for i in range(4)]
        B_gains = mkbuf("gains")
        B_xt = [mkbuf("xt%d" % i) for i in range(4)]
        B_xn = [mkbuf("xn%d" % i) for i in range(2)]
        B_hT = mkbuf("hT")
        B_const = mkbuf("const")
        B_stat = [mkbuf("stat%d" % i) for i in range(16)]
        B_junk = mkbuf("junk")
        B_misc = mkbuf("misc")
        B_state = mkbuf("state")
        B_winr = [mkbuf("winr%d" % i) for i in range(4)]
        B_kT = [mkbuf("kT%d" % i) for i in range(4)]
        B_khot = mkbuf("khot")
        B_va = [mkbuf("va%d" % i) for i in range(4)]
        B_qT = mkbuf("qT")
        B_qTa = mkbuf("qTa")
        B_pT = [mkbuf("pT%d" % i) for i in range(3)]
        B_xr = [mkbuf("xr%d" % i) for i in range(4)]
        B_gg = [mkbuf("gg%d" % i) for i in range(4)]
        B_y = mkbuf("y")
        B_ybf = mkbuf("ybf")
        B_rb = mkbuf("rb")
        B_ib = mkbuf("ib")
        B_ub = mkbuf("ub")
        B_hs = mkbuf("hs")
        B_kmean = mkbuf("kmean")
        B_Msel = mkbuf("Msel")
        B_gm = mkbuf("gm")
        B_top8 = mkbuf("top8")
        B_thr = mkbuf("thr")
        B_selb = mkbuf("selb")
        B_rden = mkbuf("rden")
        B_bcsb = mkbuf("bcsb")
        B_wdown = mkbuf("wdown")
        B_wout = mkbuf("wout")
        B_wupr = [mkbuf("wupr%d" % i) for i in range(4)]
        B_actT = mkbuf("actT")
        B_xub = [mkbuf("xub%d" % i) for i in range(2)]
        B_xgb = [mkbuf("xgb%d" % i) for i in range(2)]
        B_cu = mkbuf("cu")
        B_cg = mkbuf("cg")
        B_gl = mkbuf("gl")
        B_halo = mkbuf("halo")
        B_tmpf = mkbuf("tmpf")

        V, S, G, P = nc.vector, nc.scalar, nc.gpsimd, nc.tensor

        stat_i = [0]

        def stat():
            i = stat_i[0] % 16
            stat_i[0] += 1
            return stat_h[:, i * 4:(i + 1) * 4], B_stat[i]

        eps_ap = misc_h[:, 0:1]
        one_ap = onesf_h[:, 0:1]
        neg8sp = misc_h[:, 4:8]
        state = misc_h[:, 8:12]

        for g in range(4):
            T.dma("sp", "c", out=gains_h[:, g * D:(g + 1) * D].rearrange("p (o c) -> p o c", o=1),
                  in_=gains_d[g:g + 1, :].partition_broadcast(128), W=[B_gains])
        T.dma("sp", "c", out=pm_h[:, :].rearrange("p (o c) -> p o c", o=1),
              in_=pm_d[0:1, :].partition_broadcast(128), W=[B_const])
        T.dma("sp", "c", out=c31_h[:, :].rearrange("p (o c) -> p o c", o=1),
              in_=c31_d[0:1, :].partition_broadcast(128), W=[B_const])
        T.dma("sp", "c", out=lrucols_h[:, :], in_=lrucols_d[:, :], W=[B_const])
        T.dma("sp", "c", out=ffncols_h[:, :], in_=ffncols_d[:, :], W=[B_const])
        T.dma("sp", "c", out=xt_h[:, 0:2048], in_=btraw_d[:, :], W=[B_xt[0], B_xt[1]])
        T.dma("pool", "cc", out=ident_h[:, :], in_=ident_d[:, :], W=[B_const])
        T.dma("pool", "cc", out=wrbd_h[:, :], in_=wrbd_d[:, :], W=[B_const])
        T.dma("pool", "cc", out=wibd_h[:, :], in_=wibd_d[:, :], W=[B_const])
        T.op("dve", lambda: V.memset(onesf_h[:, :], 1.0), W=[B_misc])
        T.op("dve", lambda: V.memset(misc_h[:, :], EPS), W=[B_misc])
        for h in range(8):
            T.op("dve", lambda: V.tensor_scalar(out=btb_h[:, h * 256:(h + 1) * 256], in0=xt_h[:, h * 256:(h + 1) * 256],
                                                scalar1=c31_h[:, h:h + 1], scalar2=None, op0=ALU.subtract),
                 R=[B_xt[0], B_xt[1], B_const], W=[B_const])
        lc = lrucols_h[:, :].rearrange("p (c e) -> p c e", c=4)
        st1, bst1 = stat()
        T.op("act", lambda: S.activation(out=st1, in_=lc[:, :, 7], func=AF.Exp, scale=-1.0), R=[B_const], W=[bst1])
        T.op("act", lambda: S.activation(out=st1, in_=st1, func=AF.Ln, bias=one_ap, scale=1.0), R=[B_misc], W=[bst1])
        T.op("dve", lambda: V.tensor_scalar(out=neg8sp, in0=st1, scalar1=-8.0, scalar2=None, op0=ALU.mult),
             R=[bst1], W=[B_misc])

        gpre_mix = gains_h[:, 0 * D:1 * D]
        gpost_mix = gains_h[:, 1 * D:2 * D]
        gpre_ffn = gains_h[:, 2 * D:3 * D]
        gpost_ffn = gains_h[:, 3 * D:4 * D]

        def rstd_from(ssA, bA, ssB=None, bB=None):
            t, bt = stat()
            if ssB is None:
                T.op("dve", lambda: V.tensor_scalar(out=t[:, 0:1], in0=ssA, scalar1=1.0 / D, scalar2=None, op0=ALU.mult),
                     R=[bA], W=[bt])
            else:
                T.op("dve", lambda: V.tensor_scalar(out=t[:, 0:1], in0=ssA, scalar1=ssB, scalar2=1.0 / D,
                                                    op0=ALU.add, op1=ALU.mult), R=[bA, bB], W=[bt])
            T.op("act", lambda: S.activation(out=t[:, 1:2], in_=t[:, 0:1], func=AF.Sqrt, bias=eps_ap, scale=1.0),
                 R=[B_misc], W=[bt])
            T.op("dve", lambda: V.reciprocal(out=t[:, 2:3], in_=t[:, 1:2]), W=[bt])
            return t[:, 2:3], bt

        xn_i = [0]

        def norm_to_hT(sub, gain_ap):
            xs = xt_h[:, sub * D:(sub + 1) * D]
            ss, bss = stat()
            T.op("act", lambda: S.activation(out=junk_h[:, :], in_=xs, func=AF.Square, accum_out=ss[:, 0:1]),
                 R=[B_xt[sub]], W=[B_junk, bss])
            rs, brs = rstd_from(ss[:, 0:1], bss)
            i = xn_i[0] % 2
            xn_i[0] += 1
            T.op("dve", lambda: V.scalar_tensor_tensor(out=xn_h[i][:, :], in0=xs, scalar=rs, in1=gain_ap,
                                                       op0=ALU.mult, op1=ALU.mult),
                 R=[B_xt[sub], brs, B_gains], W=[B_xn[i]])
            for k in range(8):
                T.op("pe", lambda: P.transpose(ps7b[:, k * 128:(k + 1) * 128], xn_h[i][:, k * 128:(k + 1) * 128],
                                               ident_h[:, :]),
                     R=[B_xn[i], B_const], W=[B_ps[7]], inc=(k == 7))
            T.op("act", lambda: S.activation(out=hT_v[:, :, sub * 128:(sub + 1) * 128],
                                             in_=ps7b[:, :].rearrange("p (k t) -> p k t", k=8), func=AF.Copy),
                 R=[B_ps[7]], W=[B_hT])

        def norm4_A():
            sss = []
            for sub in range(4):
                xs = xt_h[:, sub * D:(sub + 1) * D]
                ss, bss = stat()
                T.op("act", lambda: S.activation(out=junk_h[:, :], in_=xs, func=AF.Square, accum_out=ss[:, 0:1]),
                     R=[B_xt[sub]], W=[B_junk, bss])
                sss.append((ss, bss))
            ts_ = []
            for sub in range(4):
                ss, bss = sss[sub]
                t, bt = stat()
                T.op("dve", lambda: V.tensor_scalar(out=t[:, 0:1], in0=ss[:, 0:1], scalar1=1.0 / D, scalar2=None, op0=ALU.mult),
                     R=[bss], W=[bt])
                ts_.append((t, bt))
            for sub in range(4):
                t, bt = ts_[sub]
                T.op("act", lambda: S.activation(out=t[:, 1:2], in_=t[:, 0:1], func=AF.Sqrt, bias=eps_ap, scale=1.0),
                     R=[B_misc], W=[bt])
            for sub in range(4):
                t, bt = ts_[sub]
                T.op("dve", lambda: V.reciprocal(out=t[:, 2:3], in_=t[:, 1:2]), W=[bt])
            return ts_

        def norm4_B(ts_, gain_ap):
            for sub in range(4):
                t, bt = ts_[sub]
                xs = xt_h[:, sub * D:(sub + 1) * D]
                i = xn_i[0] % 2
                xn_i[0] += 1
                T.op("dve", lambda: V.scalar_tensor_tensor(out=xn_h[i][:, :], in0=xs, scalar=t[:, 2:3], in1=gain_ap,
                                                           op0=ALU.mult, op1=ALU.mult),
                     R=[B_xt[sub], bt, B_gains], W=[B_xn[i]])
                for k in range(8):
                    T.op("pe", lambda: P.transpose(ps7b[:, k * 128:(k + 1) * 128], xn_h[i][:, k * 128:(k + 1) * 128],
                                                   ident_h[:, :]),
                         R=[B_xn[i], B_const], W=[B_ps[7]], inc=(k == 7))
                T.op("act", lambda: S.activation(out=hT_v[:, :, sub * 128:(sub + 1) * 128],
                                                 in_=ps7b[:, :].rearrange("p (k t) -> p k t", k=8), func=AF.Copy),
                     R=[B_ps[7]], W=[B_hT])

        def load_x(tok0):
            for sub in range(4):
                T.dma("sp", "x%d" % sub, out=xt_h[:, sub * D:(sub + 1) * D],
                      in_=x_d[tok0 + sub * 128: tok0 + (sub + 1) * 128, :], W=[B_xt[sub]])

        inb = [0]

        def in_bank():
            b = inb[0] % 2
            inb[0] += 1
            return b

        def mixer_phase(s):
            for h in range(8):
                T.dma("pool", "cc", out=kT[64:72, h, :], in_=khot_d[:, :], W=[B_khot])
            T.op("pool", lambda: G.memset(vaug[:, :, :, 64:128].rearrange("p a b c -> p (a b) c"), 1.0), W=B_va)
            T.op("pool", lambda: G.memset(Msel.rearrange("p h c -> p (h c)"), 0.0), W=[B_Msel])
            T.op("pool", lambda: G.memset(kmean.rearrange("p h j -> p (h j)"), 0.0), W=[B_kmean])
            T.op("pool", lambda: G.memset(xr.rearrange("p c t -> p (c t)"), 0.0), W=B_xr)
            T.op("pool", lambda: G.memset(state, 0.0), W=[B_state])
            chunks = [(t, c) for t in range(4) for c in range(10)]
            dptr = [0]

            def issue_chunk():
                if dptr[0] >= len(chunks):
                    return
                gidx = dptr[0]
                _, c = chunks[gidx]
                slot = gidx % 4
                T.dma("pool", "winr%d" % slot, out=winr[slot], in_=w_in_v[:, :, c * 256:(c + 1) * 256], W=[B_winr[slot]])
                dptr[0] += 1

            issue_chunk()
            issue_chunk()
            issue_chunk()
            load_x(s * SEQ)
            norm4_B(norm4_A(), gpre_mix)
            load_x(s * SEQ + 512)
            gidx = 0
            nxt = [None]
            for t in range(4):
                tcol = slice(t * 512, (t + 1) * 512)
                ck(1)
                for c in range(10):
                    ck(1.0 + 0.05 * c + 0.01)
                    issue_chunk()
                    slot = gidx % 4
                    W_ = winr[slot]
                    BW = B_winr[slot]
                    gidx += 1
                    if c < 4:
                        for hh in range(4):
                            h = (c % 2) * 4 + hh
                            b = in_bank()
                            for k in range(8):
                                T.op("pe", lambda: P.matmul(ps[b][0:64, :], lhsT=W_[:, k, hh * 64:(hh + 1) * 64],
                                                            rhs=hT_v[:, k, :], start=(k == 0), stop=(k == 7)),
                                     R=[BW, B_hT], W=[B_ps[b]], inc=(k == 7))
                            if c < 2:
                                T.op("act", lambda: S.activation(out=qT[0:64, h, :], in_=ps[b][0:64, :], func=AF.Copy,
                                                                 scale=0.125), R=[B_ps[b]], W=[B_qT])
                            else:
                                ks, bks = stat()
                                for jb in range(2):
                                    T.op("act", lambda: S.activation(out=kT[0:64, h, t * 512 + jb * 256: t * 512 + (jb + 1) * 256],
                                                                     in_=ps[b][0:64, jb * 256:(jb + 1) * 256], func=AF.Copy,
                                                                     accum_out=ks[0:64, jb:jb + 1]),
                                         R=[B_ps[b]], W=[B_kT[t], bks])
                                T.op("dve", lambda: V.tensor_scalar(out=kmean[0:64, h, 2 * t:2 * t + 2], in0=ks[0:64, 0:2],
                                                                    scalar1=1.0 / 256, scalar2=None, op0=ALU.mult),
                                     R=[bks], W=[B_kmean])
                    elif c < 6:
                        cp = c - 4
                        for sub in range(4):
                            b = in_bank()
                            for k in range(8):
                                T.op("pe", lambda: P.matmul(ps[b][:, 0:256], lhsT=hT_v[:, k, sub * 128:(sub + 1) * 128],
                                                            rhs=W_[:, k, :], start=(k == 0), stop=(k == 7)),
                                     R=[BW, B_hT], W=[B_ps[b]], inc=(k == 7))
                            pv4 = ps[b][:, 0:256].rearrange("p (a e c) -> p a e c", a=2, e=2)
                            kt = 4 * t + sub
                            T.op("act", lambda: S.activation(out=vaug[:, kt, 2 * cp:2 * cp + 2, 0:64], in_=pv4[:, :, 0, :],
                                                             func=AF.Copy), R=[B_ps[b]], W=[B_va[t]])
                            T.op("dve", lambda: V.tensor_copy(out=vaug[:, kt, 2 * cp:2 * cp + 2, 128:192], in_=pv4[:, :, 1, :]),
                                 R=[B_ps[b]], W=[B_va[t]])
                    else:
                        for cc in range(2):
                            ch = 2 * ((c - 6) % 2) + cc
                            b = in_bank()
                            for k in range(8):
                                T.op("pe", lambda: P.matmul(ps[b][:, :], lhsT=W_[:, k, cc * 128:(cc + 1) * 128],
                                                            rhs=hT_v[:, k, :], start=(k == 0), stop=(k == 7)),
                                     R=[BW, B_hT], W=[B_ps[b]], inc=(k == 7))
                            if c < 8:
                                T.op("dve", lambda: V.tensor_copy(out=xr[:, ch, 3:515], in_=ps[b][:, :]),
                                     R=[B_ps[b]], W=[B_xr[ch]])
                            else:
                                T.op("act", lambda: S.activation(out=gg[:, ch, :], in_=ps[b][:, :], func=AF.Gelu_apprx_tanh),
                                     R=[B_ps[b]], W=[B_gg[ch]])
                ck(2)
                for sub in range(4):
                    own = 2 * t + sub // 2
                    scol = slice(sub * 128, (sub + 1) * 128)
                    for h in range(8):
                        T.op("pe", lambda: P.matmul(ps[6][:, sub * 64 + h * 8: sub * 64 + h * 8 + 8], lhsT=qT[0:64, h, scol],
                                                    rhs=kmean[0:64, h, :], start=True, stop=True),
                             R=[B_qT, B_kmean], W=[B_ps[6]], inc=(h == 7))
                    T.op("dve", lambda: V.tensor_tensor(out=gm, in0=ps[6][:, sub * 64:(sub + 1) * 64],
                                                        in1=pm_h[:, own * 64:(own + 1) * 64], op=ALU.add),
                         R=[B_ps[6], B_const], W=[B_gm])
                    for h in range(8):
                        T.op("dve", lambda: V.max(out=top8[:, h * 8:(h + 1) * 8], in_=gm[:, h * 8:(h + 1) * 8]),
                             R=[B_gm], W=[B_top8])
                    T.op("dve", lambda: V.tensor_scalar(out=thr, in0=top8.rearrange("p (h j) -> p h j", h=8)[:, :, 3],
                                                        scalar1=-1e29, scalar2=None, op0=ALU.max), R=[B_top8], W=[B_thr])
                    T.op("dve", lambda: V.tensor_tensor(out=selb.rearrange("p (h j) -> p h j", h=8),
                                                        in0=gm.rearrange("p (h j) -> p h j", h=8),
                                                        in1=thr.unsqueeze(2).to_broadcast([128, 8, 8]), op=ALU.is_ge),
                         R=[B_gm, B_thr], W=[B_selb])
                    T.op("dve", lambda: V.tensor_scalar(out=Msel[:, :, 64:72], in0=selb.rearrange("p (h j) -> p h j", h=8),
                                                        scalar1=-NEG, scalar2=NEG, op0=ALU.mult, op1=ALU.add),
                         R=[B_selb], W=[B_Msel])
                    for h in range(8):
                        T.op("pe", lambda: P.transpose(ps7b[0:72, h * 128:(h + 1) * 128], Msel[:, h, :], ident_h[:, :]),
                             R=[B_Msel, B_const], W=[B_ps[7]], inc=(h == 7))
                    T.op("act", lambda: S.activation(out=qT[64:72, :, scol],
                                                     in_=ps7b[64:72, :].rearrange("p (h t) -> p h t", h=8), func=AF.Copy),
                         R=[B_ps[7]], W=[B_qTa])
                ck(3)
                def lru_A(ch):
                    lcol = lambda e: lc[:, ch, e:e + 1]
                    T.op("dve", lambda: V.tensor_scalar(out=ybuf, in0=xr[:, ch, 3:515], scalar1=lcol(3), scalar2=lcol(4),
                                                        op0=ALU.mult, op1=ALU.add), R=[B_xr[ch], B_const], W=[B_y])
                    for j in (2, 1, 0):
                        T.op("dve", lambda: V.scalar_tensor_tensor(out=ybuf, in0=xr[:, ch, j:j + 512], scalar=lcol(j), in1=ybuf,
                                                                   op0=ALU.mult, op1=ALU.add), R=[B_xr[ch], B_const], W=[B_y])
                    T.op("pool", lambda: G.tensor_copy(out=xr[:, ch, 0:3], in_=xr[:, ch, 512:515]), W=[B_xr[ch]])
                    T.op("act", lambda: S.activation(out=ybf, in_=ybuf, func=AF.Copy), R=[B_y], W=[B_ybf])

                def lru_B(ch):
                    lcol = lambda e: lc[:, ch, e:e + 1]
                    T.op("pe", lambda: P.matmul(ps[0][:, :], lhsT=wrbd_h[:, ch * 128:(ch + 1) * 128], rhs=ybf, start=True, stop=True),
                         R=[B_ybf, B_const], W=[B_ps[0]])
                    T.op("pe", lambda: P.matmul(ps[1][:, :], lhsT=wibd_h[:, ch * 128:(ch + 1) * 128], rhs=ybf, start=True, stop=True),
                         R=[B_ybf, B_const], W=[B_ps[1]])
                    T.op("act", lambda: S.activation(out=rb, in_=ps[0][:, :], func=AF.Sigmoid, bias=lcol(5), scale=1.0),
                         R=[B_ps[0], B_const], W=[B_rb])
                    T.op("act", lambda: S.activation(out=ib, in_=ps[1][:, :], func=AF.Sigmoid, bias=lcol(6), scale=1.0),
                         R=[B_ps[1], B_const], W=[B_ib])
                    T.op("act", lambda: S.activation(out=rb, in_=rb, func=AF.Exp, scale=neg8sp[:, ch:ch + 1]),
                         R=[B_misc], W=[B_rb])
                    T.op("dve", lambda: V.tensor_tensor(out=ub, in0=ib, in1=ybuf, op=ALU.mult), R=[B_ib, B_y], W=[B_ub])
                    T.op("dve", lambda: V.tensor_tensor(out=ib, in0=rb, in1=rb, op=ALU.mult), R=[B_rb], W=[B_ib])
                    T.op("act", lambda: S.activation(out=ib, in_=ib, func=AF.Sqrt, bias=one_ap, scale=-1.0),
                         R=[B_misc], W=[B_ib])
                    T.op("dve", lambda: V.tensor_tensor(out=ub, in0=ub, in1=ib, op=ALU.mult), R=[B_ib], W=[B_ub])
                    T.op("dve", lambda: V.tensor_tensor_scan(out=hs, data0=rb, data1=ub, initial=state[:, ch:ch + 1],
                                                             op0=ALU.mult, op1=ALU.add), R=[B_rb, B_ub, B_state], W=[B_hs])
                    T.op("dve", lambda: V.tensor_copy(out=state[:, ch:ch + 1], in_=hs[:, 511:512]), R=[B_hs], W=[B_state])
                    T.op("pool", lambda: G.tensor_tensor(out=E_v[:, 4 + ch, tcol], in0=hs, in1=gg[:, ch, :], op=ALU.mult),
                         R=[B_hs, B_gg[ch]], W=[B_E[t]])

                lru_A(0)
                ck(4)
                pend = None

                def finalize(h, b):
                    odd = h % 2
                    if odd:
                        dp, np_ = slice(0, 1), slice(64, 128)
                    else:
                        dp, np_ = slice(64, 65), slice(0, 64)
                    T.op("dve", lambda: V.reciprocal(out=rden[dp, :], in_=ps[b][dp, :]), R=[B_ps[b]], W=[B_rden])
                    if odd:
                        T.op("pe", lambda: P.matmul(ps[6][0:128, :], lhsT=onesf_h[0:1, 0:128], rhs=rden[0:1, :], start=True, stop=True),
                             R=[B_rden, B_misc], W=[B_ps[6]])
                    else:
                        T.op("pe", lambda: P.matmul(ps[6][0:64, :], lhsT=onesf_h[64:65, 0:64], rhs=rden[64:65, :], start=True, stop=True),
                             R=[B_rden, B_misc], W=[B_ps[6]])
                    T.op("act", lambda: S.activation(out=bcsb[np_, :], in_=ps[6][np_, :], func=AF.Copy), R=[B_ps[6]], W=[B_bcsb])
                    T.op("dve", lambda: V.tensor_tensor(out=E_v[np_, h // 2, tcol], in0=ps[b][np_, :], in1=bcsb[np_, :], op=ALU.mult),
                         R=[B_ps[b], B_bcsb], W=[B_E[t]])

                nkt = 4 * t + 4
                items = [(h, ki) for h in range(8) for ki in range(nkt)]

                def geom(ki):
                    r = ki - 4 * t
                    c0 = 0 if r < 0 else 128 * r
                    return r, c0

                def emit_qk(idx):
                    h, ki = items[idx]
                    r, c0 = geom(ki)
                    sb_ = 2 + (idx % 2)
                    need_bias = r >= -1
                    T.op("pe", lambda: P.matmul(ps[sb_][:, c0:512], lhsT=kT[0:72, h, ki * 128:(ki + 1) * 128],
                                                rhs=qT[0:72, h, c0:512], start=True, stop=not need_bias),
                         R=[B_kT[ki // 4], B_khot, B_qT, B_qTa], W=[B_ps[sb_]], inc=not need_bias)
                    if need_bias:
                        if r == -1:
                            bc0, bc1, bb0 = 0, 128, 128
                        elif r == 3:
                            bc0, bc1, bb0 = 384, 512, 0
                        else:
                            bc0, bc1, bb0 = 128 * r, 128 * r + 256, 0
                        T.op("pe", lambda: P.matmul(ps[sb_][:, bc0:bc1], lhsT=ident_h[:, :],
                                                    rhs=btb_h[:, h * 256 + bb0: h * 256 + bb0 + (bc1 - bc0)],
                                                    start=False, stop=True),
                             R=[B_const], W=[B_ps[sb_]])

                def emit_exp_pv(idx):
                    h, ki = items[idx]
                    r, c0 = geom(ki)
                    sb_ = 2 + (idx % 2)
                    pt_i = idx % 3
                    pvb = 4 + (h % 2)
                    pr = h // 2
                    T.op("act", lambda: S.activation(out=pT[pt_i][:, c0:512], in_=ps[sb_][:, c0:512], func=AF.Exp,
                                                     bias=c31_h[:, h:h + 1], scale=1.0),
                         R=[B_ps[sb_], B_const], W=[B_pT[pt_i]])
                    if h % 2 == 0:
                        T.op("pe", lambda: P.matmul(ps[pvb][0:65, c0:512], lhsT=vaug[:, ki, pr, 0:65], rhs=pT[pt_i][:, c0:512],
                                                    start=(ki == 0), stop=(ki == nkt - 1)),
                             R=[B_va[ki // 4], B_pT[pt_i]], W=[B_ps[pvb]], inc=(ki == nkt - 1))
                    else:
                        T.op("pe", lambda: P.matmul(ps[pvb][0:128, c0:512], lhsT=vaug[:, ki, pr, 64:192], rhs=pT[pt_i][:, c0:512],
                                                    start=(ki == 0), stop=(ki == nkt - 1)),
                             R=[B_va[ki // 4], B_pT[pt_i]], W=[B_ps[pvb]], inc=(ki == nkt - 1))

                pend = None
                emit_qk(0)
                for idx, (h, ki) in enumerate(items):
                    if idx + 1 < len(items):
                        emit_qk(idx + 1)
                    emit_exp_pv(idx)
                    if t < 3 and h == 5 and ki == 2:
                        nxt[0] = norm4_A()
                    if t < 3 and h == 6 and ki == 2:
                        norm4_B(nxt[0], gpre_mix)
                        if t + 2 <= 3:
                            load_x(s * SEQ + (t + 2) * 512)
                    if ki == 1:
                        if h % 2 == 0:
                            lru_B(h // 2)
                        elif h // 2 + 1 < 4:
                            lru_A(h // 2 + 1)
                    if ki == nkt // 2 and pend is not None:
                        finalize(*pend)
                        pend = None
                    if ki == nkt - 1:
                        pend = (h, 4 + (h % 2))
                finalize(*pend)
                ck(5)

        def ffn_phase(s):
            for q4 in range(4):
                T.dma("pool", "wout", out=wout[:, 2 * q4:2 * q4 + 2, :], in_=w_out_v[:, 2 * q4:2 * q4 + 2, :], W=[B_wout])
            T.op("pool", lambda: G.memset(halo.rearrange("p j t -> p (j t)"), 0.0), W=[B_halo])
            chunks = [(t, j) for t in range(4) for j in range(22)]
            dptr = [0]

            def issue_chunk():
                if dptr[0] >= len(chunks):
                    return
                gidx = dptr[0]
                _, j = chunks[gidx]
                slot = gidx % 4
                T.dma("pool", "wupr%d" % slot, out=wupr[slot][:, :, 0:128], in_=w_up_v[:, :, j * 128:(j + 1) * 128],
                      W=[B_wupr[slot]])
                T.dma("pool", "wupr%d" % slot, out=wupr[slot][:, :, 128:256],
                      in_=w_up_v[:, :, DFF + j * 128: DFF + (j + 1) * 128], W=[B_wupr[slot]])
                dptr[0] += 1

            issue_chunk()
            issue_chunk()
            issue_chunk()
            for q11 in range(11):
                T.dma("pool", "wdown", out=wdown[:, 2 * q11:2 * q11 + 2, :], in_=w_down_v[:, 2 * q11:2 * q11 + 2, :], W=[B_wdown])
            gidx = 0
            ob = [0]

            def post_norm_residual(pa, pb, Ba, Bb, gain_ap, sub):
                ss, bss = stat()
                T.op("act", lambda: S.activation(out=junk_h[:, 0:512], in_=pa, func=AF.Square, accum_out=ss[:, 0:1]),
                     R=[Ba], W=[B_junk, bss])
                T.op("act", lambda: S.activation(out=junk_h[:, 512:1024], in_=pb, func=AF.Square, accum_out=ss[:, 1:2]),
                     R=[Bb], W=[B_junk, bss])
                rs, brs = rstd_from(ss[:, 0:1], bss, ss[:, 1:2], bss)
                T.op("dve", lambda: V.scalar_tensor_tensor(out=pa, in0=pa, scalar=rs, in1=gain_ap[:, 0:512],
                                                           op0=ALU.mult, op1=ALU.mult), R=[brs, B_gains], W=[Ba])
                T.op("dve", lambda: V.scalar_tensor_tensor(out=pb, in0=pb, scalar=rs, in1=gain_ap[:, 512:1024],
                                                           op0=ALU.mult, op1=ALU.mult), R=[brs, B_gains], W=[Bb])
                xs = xt_h[:, sub * D:(sub + 1) * D]
                T.op("dve", lambda: V.tensor_tensor(out=xs[:, 0:512], in0=xs[:, 0:512], in1=pa, op=ALU.add), R=[Ba], W=[B_xt[sub]])
                T.op("dve", lambda: V.tensor_tensor(out=xs[:, 512:1024], in0=xs[:, 512:1024], in1=pb, op=ALU.add), R=[Bb], W=[B_xt[sub]])

            for t in range(4):
                tok0 = s * SEQ + t * 512
                load_x(tok0)
                def oproj(sub, o0):
                    ecol = slice(t * 512 + sub * 128, t * 512 + (sub + 1) * 128)
                    for half in range(2):
                        for k in range(8):
                            T.op("pe", lambda: P.matmul(ps[o0 + half][:, :], lhsT=E_v[:, k, ecol],
                                                        rhs=wout[:, k, half * 512:(half + 1) * 512], start=(k == 0), stop=(k == 7)),
                                 R=[B_E[t], B_wout], W=[B_ps[o0 + half]], inc=(k == 7))

                def chain(sub, o0):
                    post_norm_residual(ps[o0][:, :], ps[o0 + 1][:, :], B_ps[o0], B_ps[o0 + 1], gpost_mix, sub)

                oproj(0, 0)
                oproj(1, 2)
                chain(0, 0)
                oproj(2, 0)
                chain(1, 2)
                oproj(3, 2)
                norm_to_hT(0, gpre_ffn)
                chain(2, 0)
                norm_to_hT(1, gpre_ffn)
                chain(3, 2)
                norm_to_hT(2, gpre_ffn)
                norm_to_hT(3, gpre_ffn)
                ck(11)
                for j in range(22):
                    issue_chunk()
                    slot = gidx % 4
                    gidx += 1
                    W_ = wupr[slot]
                    BW = B_wupr[slot]
                    pu = 4 + 2 * (j % 2)
                    pg = 5 + 2 * (j % 2)
                    bi = j % 2
                    for k in range(8):
                        T.op("pe", lambda: P.matmul(ps[pu][:, :], lhsT=W_[:, k, 0:128], rhs=hT_v[:, k, :], start=(k == 0), stop=(k == 7)),
                             R=[BW, B_hT], W=[B_ps[pu]], inc=(k == 7))
                    for k in range(8):
                        T.op("pe", lambda: P.matmul(ps[pg][:, :], lhsT=W_[:, k, 128:256], rhs=hT_v[:, k, :], start=(k == 0), stop=(k == 7)),
                             R=[BW, B_hT], W=[B_ps[pg]], inc=(k == 7))
                    T.op("pool", lambda: G.tensor_copy(out=xub[bi][:, 0:2], in_=halo[:, j, :]), R=[B_halo], W=[B_xub[bi]])
                    T.op("pool", lambda: G.tensor_copy(out=xgb[bi][:, 0:2], in_=halo[:, 22 + j, :]), R=[B_halo], W=[B_xgb[bi]])
                    T.op("act", lambda: S.activation(out=xgb[bi][:, 2:514], in_=ps[pg][:, :], func=AF.Copy), R=[B_ps[pg]], W=[B_xgb[bi]])
                    T.op("act", lambda: S.activation(out=xub[bi][:, 2:514], in_=ps[pu][:, :], func=AF.Copy), R=[B_ps[pu]], W=[B_xub[bi]])
                    fc = ffncols_h[:, :].rearrange("p (j e) -> p j e", j=44)
                    def conv_first(src, bsrc, dst, bdst, jj):
                        T.op("dve", lambda: V.tensor_scalar(out=dst, in0=src[:, 2:514], scalar1=fc[:, jj, 2:3], scalar2=fc[:, jj, 3:4],
                                                            op0=ALU.mult, op1=ALU.add), R=[bsrc, B_const], W=[bdst])

                    def conv_tap(src, bsrc, dst, bdst, jj, tap):
                        T.op("dve", lambda: V.scalar_tensor_tensor(out=dst, in0=src[:, tap:tap + 512], scalar=fc[:, jj, tap:tap + 1],
                                                                   in1=dst, op0=ALU.mult, op1=ALU.add), R=[bsrc, B_const], W=[bdst])

                    ga = (xgb[bi], B_xgb[bi], cg, B_cg, 22 + j)
                    ua = (xub[bi], B_xub[bi], cu, B_cu, j)
                    T.op("act", lambda: S.activation(out=cg, in_=ps[pg][:, :], func=AF.Identity, bias=fc[:, 22 + j, 3:4],
                                                     scale=fc[:, 22 + j, 2:3]), R=[B_ps[pg], B_const], W=[B_cg])
                    T.op("act", lambda: S.activation(out=cu, in_=ps[pu][:, :], func=AF.Identity, bias=fc[:, j, 3:4],
                                                     scale=fc[:, j, 2:3]), R=[B_ps[pu], B_const], W=[B_cu])
                    conv_tap(*ga, 1)
                    conv_tap(*ua, 1)
                    conv_tap(*ga, 0)
                    T.op("act", lambda: S.activation(out=gl, in_=cg, func=AF.Gelu_apprx_tanh), R=[B_cg], W=[B_gl])
                    conv_tap(*ua, 0)
                    T.op("pool", lambda: G.tensor_copy(out=halo[:, j, :], in_=xub[bi][:, 512:514]), R=[B_xub[bi]], W=[B_halo])
                    T.op("pool", lambda: G.tensor_copy(out=halo[:, 22 + j, :], in_=xgb[bi][:, 512:514]), R=[B_xgb[bi]], W=[B_halo])
                    T.op("dve", lambda: V.tensor_tensor(out=actT[:, j, :], in0=cu, in1=gl, op=ALU.mult), R=[B_cu, B_gl], W=[B_actT])
                ck(12)
                for sub in range(4):
                    o0 = 2 * (ob[0] % 2)
                    ob[0] += 1
                    for half in range(2):
                        for j in range(22):
                            T.op("pe", lambda: P.matmul(ps[o0 + half][:, :], lhsT=actT[:, j, sub * 128:(sub + 1) * 128],
                                                        rhs=wdown[:, j, half * 512:(half + 1) * 512], start=(j == 0), stop=(j == 21)),
                                 R=[B_actT, B_wdown], W=[B_ps[o0 + half]], inc=(j == 21))
                    post_norm_residual(ps[o0][:, :], ps[o0 + 1][:, :], B_ps[o0], B_ps[o0 + 1], gpost_ffn, sub)
                    T.dma("sp", "y%d" % sub, out=y_d[tok0 + sub * 128: tok0 + (sub + 1) * 128, :],
                          in_=xt_h[:, sub * D:(sub + 1) * D], R=[B_xt[sub]])

        try:
            for s in range(2):
                T.barrier(allbufs)
                ck(0)
                mixer_phase(s)
                T.barrier(allbufs)
                ck(10)
                ffn_phase(s)
                ck(20)
        except _Stop:
            pass
        T.barrier(allbufs)
    return nc


def _bucket(d):
    n = np.maximum(d, 0)
    nf = np.maximum(n, 1).astype(np.float32)
    large = 16 + (np.log(nf / np.float32(16)) / np.float32(math.log(128 / 16)) * np.float32(16)).astype(np.int32)
    large = np.minimum(large, 31)
    return np.where(n < 16, n, large)


_NC_CACHE = {}


def kernel(x, g_pre_mix, w_in, rel_bias, w_conv_lru, b_conv_lru, w_r, b_r, w_i, b_i, lam,
           w_out, g_post_mix, g_pre_ffn, w_up, w_conv_ffn, b_conv_ffn, w_down, g_post_ffn):
    f32 = lambda a: np.ascontiguousarray(np.asarray(a, dtype=np.float32))
    x = f32(x)
    rel_bias = f32(rel_bias)
    gains = np.stack([f32(g_pre_mix)[0], f32(g_post_mix)[0], f32(g_pre_ffn)[0], f32(g_post_ffn)[0]], 0)
    cols = np.concatenate([f32(w_conv_lru)[0], f32(b_conv_lru), f32(b_r), f32(b_i), f32(lam)], 0)
    lrucols = np.ascontiguousarray(cols.reshape(8, 4, 128).transpose(2, 1, 0)).reshape(128, 32)
    wr = f32(w_r)[0]
    wi = f32(w_i)[0]
    wrbd = np.zeros((128, 4, 128), np.float32)
    wibd = np.zeros((128, 4, 128), np.float32)
    for c in range(4):
        for e in range(2):
            wrbd[e * 64:(e + 1) * 64, c, e * 64:(e + 1) * 64] = wr[2 * c + e]
            wibd[e * 64:(e + 1) * 64, c, e * 64:(e + 1) * 64] = wi[2 * c + e]
    fcols = np.concatenate([f32(w_conv_ffn)[0], f32(b_conv_ffn)], 0)
    ffncols = np.ascontiguousarray(fcols.reshape(4, 44, 128).transpose(2, 1, 0)).reshape(128, 176)
    kk = np.arange(128)[:, None]
    cc = np.arange(256)[None, :]
    dist = cc - kk
    gath = rel_bias[_bucket(dist)]
    btraw = np.where((dist >= 0)[:, :, None], gath, np.float32(NEG)).astype(np.float32)
    btraw = np.ascontiguousarray(btraw.transpose(0, 2, 1)).reshape(128, 2048)
    c31 = np.ascontiguousarray(rel_bias[31:32, :])
    khot = np.zeros((8, SEQ), np.float32)
    for j in range(8):
        khot[j, j * 256:(j + 1) * 256] = 1.0
    ident = np.eye(128, dtype=np.float32)
    pm = np.zeros((8, 8, 8), np.float32)
    for own in range(8):
        for j in range(8):
            pm[own, :, j] = 0.0 if j < own else (1e30 if j == own else -2e30)
    pm = pm.reshape(1, 512)
    shared = {
        "w_in": f32(w_in)[0], "w_out": f32(w_out)[0], "w_up": f32(w_up)[0], "w_down": f32(w_down)[0],
        "gains": gains, "lrucols": lrucols, "wrbd": wrbd.reshape(128, 512), "wibd": wibd.reshape(128, 512),
        "ffncols": ffncols, "btraw": btraw, "c31": c31, "khot": khot, "ident": ident, "pm": pm,
    }
    in_maps = []
    for c in range(NCORES):
        m = dict(shared)
        m["x"] = x[2 * c:2 * c + 2].reshape(2 * SEQ, D)
        in_maps.append(m)
    if "nc" not in _NC_CACHE:
        _NC_CACHE["nc"] = build_nc()
    nc = _NC_CACHE["nc"]
    res = run_bass_kernel_spmd(nc, in_maps, core_ids=list(range(NCORES)))
    out = np.stack([np.asarray(r["y"], dtype=np.float32).reshape(2, SEQ, D) for r in res.results], 0)
    return out.reshape(16, SEQ, D)
```

```python
import math
import os
from contextlib import ExitStack

import numpy as np
import concourse.bass as bass
import concourse.mybir as mybir
from concourse.bass_utils import run_bass_kernel_spmd

F32 = mybir.dt.float32
BF16 = mybir.dt.bfloat16
AF = mybir.ActivationFunctionType
ALU = mybir.AluOpType
AX = mybir.AxisListType

NCORES = 8
SEQ = 2048
D = 1024
DFF = 2816
NEG = -30000.0
EPS = 1e-6


class Buf:
    __slots__ = ("name", "w", "r")

    def __init__(self, name):
        self.name = name
        self.w = {}
        self.r = {}


class TK:
    def __init__(self, nc, st):
        self.nc = nc
        self.st = st
        self.E = {"pe": nc.tensor, "act": nc.scalar, "dve": nc.vector, "pool": nc.gpsimd, "sp": nc.sync}
        self.sem = {e: st.enter_context(nc.semaphore("s_" + e)) for e in ("pe", "act", "dve", "pool")}
        self.cnt = {e: 0 for e in self.sem}
        self.waited = {e: {} for e in self.E}
        self.dsem = {}
        self.dcnt = {}

    def _deps(self, R, W):
        d = {}
        for b in R:
            for k, sv in b.w.items():
                if k not in d or d[k][1] < sv[1]:
                    d[k] = sv
        for b in W:
            for src in (b.w, b.r):
                for k, sv in src.items():
                    if k not in d or d[k][1] < sv[1]:
                        d[k] = sv
        return d

    def _wait(self, e, d, skip=None):
        for k, (s, v) in d.items():
            if k == skip:
                continue
            if self.waited[e].get(k, 0) >= v:
                continue
            self.E[e].wait_ge(s, v)
            self.waited[e][k] = v

    def op(self, e, fn, R=(), W=(), inc=True):
        d = self._deps(R, W)
        self._wait(e, d, skip=("pe" if e == "pe" else None))
        ins = fn()
        if inc:
            self.cnt[e] += 1
            ins.then_inc(self.sem[e], 1)
            tv = self.cnt[e]
        else:
            tv = self.cnt[e] + 1
        t = (self.sem[e], tv)
        for b in W:
            b.w = {e: t}
            b.r = {}
        for b in R:
            if b not in W:
                b.r[e] = t
        return ins

    def dma(self, q, key, out, in_, R=(), W=()):
        if key not in self.dsem:
            self.dsem[key] = self.st.enter_context(self.nc.semaphore("d_" + key))
            self.dcnt[key] = 0
        d = self._deps(R, W)
        self._wait(q, d, skip=key)
        ins = self.E[q].dma_start(out=out, in_=in_)
        self.dcnt[key] += 16
        ins.then_inc(self.dsem[key], 16)
        t = (self.dsem[key], self.dcnt[key])
        for b in W:
            b.w = {key: t}
            b.r = {}
        for b in R:
            if b not in W:
                b.r[key] = t

    def barrier(self, bufs):
        for e in self.E:
            for k, s in self.sem.items():
                if self.cnt[k] > 0 and self.waited[e].get(k, 0) < self.cnt[k]:
                    self.E[e].wait_ge(s, self.cnt[k])
                    self.waited[e][k] = self.cnt[k]
            for k, s in self.dsem.items():
                if self.dcnt[k] > 0 and self.waited[e].get(k, 0) < self.dcnt[k]:
                    self.E[e].wait_ge(s, self.dcnt[k])
                    self.waited[e][k] = self.dcnt[k]
        for b in bufs:
            b.w = {}
            b.r = {}


class _Stop(Exception):
    pass


_STOP = float(os.environ.get('KSTOP', '99'))


def ck(n):
    if n >= _STOP:
        raise _Stop()


def build_nc():
    nc = bass.Bass("TRN2", target_bir_lowering=False)

    def din(n, s):
        return nc.dram_tensor(n, s, F32, kind="ExternalInput").ap()

    x_d = din("x", [2 * SEQ, D])
    y_d = nc.dram_tensor("y", [2 * SEQ, D], F32, kind="ExternalOutput").ap()
    w_in_d = din("w_in", [D, 2560])
    w_out_d = din("w_out", [D, D])
    w_up_d = din("w_up", [D, 2 * DFF])
    w_down_d = din("w_down", [DFF, D])
    gains_d = din("gains", [4, D])
    lrucols_d = din("lrucols", [128, 32])
    wrbd_d = din("wrbd", [128, 512])
    wibd_d = din("wibd", [128, 512])
    ffncols_d = din("ffncols", [128, 176])
    btraw_d = din("btraw", [128, 2048])
    c31_d = din("c31", [1, 8])
    khot_d = din("khot", [8, SEQ])
    ident_d = din("ident", [128, 128])
    pm_d = din("pm", [1, 512])

    w_in_v = w_in_d.rearrange("(k p) c -> p k c", p=128)
    w_out_v = w_out_d.rearrange("(k p) c -> p k c", p=128)
    w_up_v = w_up_d.rearrange("(k p) c -> p k c", p=128)
    w_down_v = w_down_d.rearrange("(j p) c -> p j c", p=128)

    with ExitStack() as st:
        T = TK(nc, st)
        allbufs = []

        def mkbuf(name):
            b = Buf(name)
            allbufs.append(b)
            return b

        def sb(name, cols, dtype):
            return nc.alloc_sbuf_tensor("sb_" + name, [128, cols], dtype)

        E_h = sb("E", 8 * SEQ, BF16)
        E_v = E_h[:, :].rearrange("p (k t) -> p k t", k=8)
        gains_h = sb("gains", 4 * D, F32)
        xt_h = sb("xt", 4 * D, F32)
        xn_h = [sb("xn%d" % i, D, BF16) for i in range(2)]
        hT_h = sb("hT", 8 * 512, BF16)
        hT_v = hT_h[:, :].rearrange("p (k t) -> p k t", k=8)
        btb_h = sb("btb", 8 * 256, BF16)
        pm_h = sb("pm", 512, F32)
        wrbd_h = sb("wrbd", 512, BF16)
        wibd_h = sb("wibd", 512, BF16)
        ident_h = sb("ident", 128, BF16)
        onesf_h = sb("onesf", 128, F32)
        lrucols_h = sb("lrucols", 32, F32)
        ffncols_h = sb("ffncols", 176, F32)
        c31_h = sb("c31", 8, F32)
        stat_h = sb("stat", 128, F32)
        junk_h = sb("junk", D, BF16)
        misc_h = sb("misc", 16, F32)
        ARENA_BYTES = 115200
        arena = sb("arena", ARENA_BYTES // 4, F32)

        class Carver:
            def __init__(self):
                self.off = 0

            def take(self, nelem, dtype):
                nbytes = nelem * (2 if dtype == BF16 else 4)
                nbytes = (nbytes + 31) // 32 * 32
                assert self.off + nbytes <= ARENA_BYTES, (self.off, nbytes)
                v = arena[:, self.off // 4:(self.off + nbytes) // 4]
                self.off += nbytes
                if dtype == BF16:
                    v = v.bitcast(BF16)
                return v[:, 0:nelem]

        cm = Carver()
        winr = [cm.take(8 * 256, BF16).rearrange("p (k c) -> p k c", k=8) for _ in range(3)]
        kT = cm.take(8 * SEQ, BF16).rearrange("p (h t) -> p h t", h=8)
        vaug = cm.take(16 * 4 * 192, BF16).rearrange("p (a b c) -> p a b c", a=16, b=4)
        qT = cm.take(8 * 512, BF16).rearrange("p (h t) -> p h t", h=8)
        pT = [cm.take(512, BF16) for _ in range(3)]
        xr = cm.take(4 * 516, F32).rearrange("p (c t) -> p c t", c=4)
        gg = cm.take(4 * 512, BF16).rearrange("p (c t) -> p c t", c=4)
        ybuf = cm.take(512, F32)
        ybf = cm.take(512, BF16)
        rb = cm.take(512, F32)
        ib = cm.take(512, F32)
        ub = cm.take(512, F32)
        hs = cm.take(512, F32)
        kmean = cm.take(64, BF16).rearrange("p (h j) -> p h j", h=8)
        Msel = cm.take(8 * 72, BF16).rearrange("p (h c) -> p h c", h=8)
        gm = cm.take(64, F32)
        top8 = cm.take(64, F32)
        thr = cm.take(8, F32)
        selb = cm.take(64, F32)
        rden = cm.take(512, F32)
        bcsb = cm.take(512, F32)
        cf = Carver()
        wdown = cf.take(22 * D, BF16).rearrange("p (j c) -> p j c", j=22)
        wout = cf.take(8 * D, BF16).rearrange("p (k c) -> p k c", k=8)
        wupr = [cf.take(8 * 256, BF16).rearrange("p (k c) -> p k c", k=8) for _ in range(3)]
        actT = cf.take(22 * 512, BF16).rearrange("p (j t) -> p j t", j=22)
        xub = [cf.take(514, F32) for _ in range(2)]
        xgb = [cf.take(514, F32) for _ in range(2)]
        cu = cf.take(512, F32)
        cg = cf.take(512, F32)
        gl = cf.take(512, F32)
        halo = cf.take(88, F32).rearrange("p (j t) -> p j t", j=44)
        tmpf = cf.take(D, F32)

        ps = [nc.alloc_psum_tensor("ps%d" % i, [128, 512], F32) for i in range(8)]
        ps7b = ps[7][:, :].bitcast(BF16)

        B_ps = [mkbuf("ps%d" % i) for i in range(8)]
        B_E = [mkbuf("E%d" % i) for i in range(4)]
        B_gains = mkbuf("gains")
        B_xt = [mkbuf("xt%d" % i) for i in range(4)]
        B_xn = [mkbuf("xn%d" % i) for i in range(2)]
        B_hT = mkbuf("hT")
        B_const = mkbuf("const")
        B_stat = [mkbuf("stat%d" % i) for i in range(16)]
        B_junk = mkbuf("junk")
        B_misc = mkbuf("misc")
        B_state = mkbuf("state")
        B_winr = [mkbuf("winr%d" % i) for i in range(3)]
        B_kT = [mkbuf("kT%d" % i) for i in range(4)]
        B_khot = mkbuf("khot")
        B_va = [mkbuf("va%d" % i) for i in range(4)]
        B_qT = mkbuf("qT")
        B_qTa = mkbuf("qTa")
        B_pT = [mkbuf("pT%d" % i) for i in range(3)]
        B_xr = [mkbuf("xr%d" % i) for i in range(4)]
        B_gg = [mkbuf("gg%d" % i) for i in range(4)]
        B_y = mkbuf("y")
        B_ybf = mkbuf("ybf")
        B_rb = mkbuf("rb")
        B_ib = mkbuf("ib")
        B_ub = mkbuf("ub")
        B_hs = mkbuf("hs")
        B_kmean = mkbuf("kmean")
        B_Msel = mkbuf("Msel")
        B_gm = mkbuf("gm")
        B_top8 = mkbuf("top8")
        B_thr = mkbuf("thr")
        B_selb = mkbuf("selb")
        B_rden = mkbuf("rden")
        B_bcsb = mkbuf("bcsb")
        B_wdown = mkbuf("wdown")
        B_wout = mkbuf("wout")
        B_wupr = [mkbuf("wupr%d" % i) for i in range(3)]
        B_actT = mkbuf("actT")
        B_xub = [mkbuf("xub%d" % i) for i in range(2)]
        B_xgb = [mkbuf("xgb%d" % i) for i in range(2)]
        B_cu = mkbuf("cu")
        B_cg = mkbuf("cg")
        B_gl = mkbuf("gl")
        B_halo = mkbuf("halo")
        B_tmpf = mkbuf("tmpf")

        V, S, G, P = nc.vector, nc.scalar, nc.gpsimd, nc.tensor

        stat_i = [0]

        def stat():
            i = stat_i[0] % 16
            stat_i[0] += 1
            return stat_h[:, i * 4:(i + 1) * 4], B_stat[i]

        eps_ap = misc_h[:, 0:1]
        one_ap = onesf_h[:, 0:1]
        neg8sp = misc_h[:, 4:8]
        state = misc_h[:, 8:12]

        for g in range(4):
            T.dma("sp", "c", out=gains_h[:, g * D:(g + 1) * D].rearrange("p (o c) -> p o c", o=1),
                  in_=gains_d[g:g + 1, :].partition_broadcast(128), W=[B_gains])
        T.dma("sp", "c", out=pm_h[:, :].rearrange("p (o c) -> p o c", o=1),
              in_=pm_d[0:1, :].partition_broadcast(128), W=[B_const])
        T.dma("sp", "c", out=c31_h[:, :].rearrange("p (o c) -> p o c", o=1),
              in_=c31_d[0:1, :].partition_broadcast(128), W=[B_const])
        T.dma("sp", "c", out=lrucols_h[:, :], in_=lrucols_d[:, :], W=[B_const])
        T.dma("sp", "c", out=ffncols_h[:, :], in_=ffncols_d[:, :], W=[B_const])
        T.dma("sp", "c", out=xt_h[:, 0:2048], in_=btraw_d[:, :], W=[B_xt[0], B_xt[1]])
        T.dma("pool", "cc", out=ident_h[:, :], in_=ident_d[:, :], W=[B_const])
        T.dma("pool", "cc", out=wrbd_h[:, :], in_=wrbd_d[:, :], W=[B_const])
        T.dma("pool", "cc", out=wibd_h[:, :], in_=wibd_d[:, :], W=[B_const])
        T.op("dve", lambda: V.memset(onesf_h[:, :], 1.0), W=[B_misc])
        T.op("dve", lambda: V.memset(misc_h[:, :], EPS), W=[B_misc])
        for h in range(8):
            T.op("dve", lambda: V.tensor_scalar(out=btb_h[:, h * 256:(h + 1) * 256], in0=xt_h[:, h * 256:(h + 1) * 256],
                                                scalar1=c31_h[:, h:h + 1], scalar2=None, op0=ALU.subtract),
                 R=[B_xt[0], B_xt[1], B_const], W=[B_const])
        lc = lrucols_h[:, :].rearrange("p (c e) -> p c e", c=4)
        st1, bst1 = stat()
        T.op("act", lambda: S.activation(out=st1, in_=lc[:, :, 7], func=AF.Exp, scale=-1.0), R=[B_const], W=[bst1])
        T.op("act", lambda: S.activation(out=st1, in_=st1, func=AF.Ln, bias=one_ap, scale=1.0), R=[B_misc], W=[bst1])
        T.op("dve", lambda: V.tensor_scalar(out=neg8sp, in0=st1, scalar1=-8.0, scalar2=None, op0=ALU.mult),
             R=[bst1], W=[B_misc])

        gpre_mix = gains_h[:, 0 * D:1 * D]
        gpost_mix = gains_h[:, 1 * D:2 * D]
        gpre_ffn = gains_h[:, 2 * D:3 * D]
        gpost_ffn = gains_h[:, 3 * D:4 * D]

        def rstd_from(ssA, bA, ssB=None, bB=None):
            t, bt = stat()
            if ssB is None:
                T.op("dve", lambda: V.tensor_scalar(out=t[:, 0:1], in0=ssA, scalar1=1.0 / D, scalar2=None, op0=ALU.mult),
                     R=[bA], W=[bt])
            else:
                T.op("dve", lambda: V.tensor_scalar(out=t[:, 0:1], in0=ssA, scalar1=ssB, scalar2=1.0 / D,
                                                    op0=ALU.add, op1=ALU.mult), R=[bA, bB], W=[bt])
            T.op("act", lambda: S.activation(out=t[:, 1:2], in_=t[:, 0:1], func=AF.Sqrt, bias=eps_ap, scale=1.0),
                 R=[B_misc], W=[bt])
            T.op("dve", lambda: V.reciprocal(out=t[:, 2:3], in_=t[:, 1:2]), W=[bt])
            return t[:, 2:3], bt

        xn_i = [0]

        def norm_to_hT(sub, gain_ap):
            xs = xt_h[:, sub * D:(sub + 1) * D]
            ss, bss = stat()
            T.op("act", lambda: S.activation(out=junk_h[:, :], in_=xs, func=AF.Square, accum_out=ss[:, 0:1]),
                 R=[B_xt[sub]], W=[B_junk, bss])
            rs, brs = rstd_from(ss[:, 0:1], bss)
            i = xn_i[0] % 2
            xn_i[0] += 1
            T.op("dve", lambda: V.scalar_tensor_tensor(out=xn_h[i][:, :], in0=xs, scalar=rs, in1=gain_ap,
                                                       op0=ALU.mult, op1=ALU.mult),
                 R=[B_xt[sub], brs, B_gains], W=[B_xn[i]])
            for k in range(8):
                T.op("pe", lambda: P.transpose(ps7b[:, k * 128:(k + 1) * 128], xn_h[i][:, k * 128:(k + 1) * 128],
                                               ident_h[:, :]),
                     R=[B_xn[i], B_const], W=[B_ps[7]], inc=(k == 7))
            T.op("act", lambda: S.activation(out=hT_v[:, :, sub * 128:(sub + 1) * 128],
                                             in_=ps7b[:, :].rearrange("p (k t) -> p k t", k=8), func=AF.Copy),
                 R=[B_ps[7]], W=[B_hT])

        def norm4_A():
            sss = []
            for sub in range(4):
                xs = xt_h[:, sub * D:(sub + 1) * D]
                ss, bss = stat()
                T.op("act", lambda: S.activation(out=junk_h[:, :], in_=xs, func=AF.Square, accum_out=ss[:, 0:1]),
                     R=[B_xt[sub]], W=[B_junk, bss])
                sss.append((ss, bss))
            ts_ = []
            for sub in range(4):
                ss, bss = sss[sub]
                t, bt = stat()
                T.op("dve", lambda: V.tensor_scalar(out=t[:, 0:1], in0=ss[:, 0:1], scalar1=1.0 / D, scalar2=None, op0=ALU.mult),
                     R=[bss], W=[bt])
                ts_.append((t, bt))
            for sub in range(4):
                t, bt = ts_[sub]
                T.op("act", lambda: S.activation(out=t[:, 1:2], in_=t[:, 0:1], func=AF.Sqrt, bias=eps_ap, scale=1.0),
                     R=[B_misc], W=[bt])
            for sub in range(4):
                t, bt = ts_[sub]
                T.op("dve", lambda: V.reciprocal(out=t[:, 2:3], in_=t[:, 1:2]), W=[bt])
            return ts_

        def norm4_B(ts_, gain_ap):
            for sub in range(4):
                t, bt = ts_[sub]
                xs = xt_h[:, sub * D:(sub + 1) * D]
                i = xn_i[0] % 2
                xn_i[0] += 1
                T.op("dve", lambda: V.scalar_tensor_tensor(out=xn_h[i][:, :], in0=xs, scalar=t[:, 2:3], in1=gain_ap,
                                                           op0=ALU.mult, op1=ALU.mult),
                     R=[B_xt[sub], bt, B_gains], W=[B_xn[i]])
                for k in range(8):
                    T.op("pe", lambda: P.transpose(ps7b[:, k * 128:(k + 1) * 128], xn_h[i][:, k * 128:(k + 1) * 128],
                                                   ident_h[:, :]),
                         R=[B_xn[i], B_const], W=[B_ps[7]], inc=(k == 7))
                T.op("act", lambda: S.activation(out=hT_v[:, :, sub * 128:(sub + 1) * 128],
                                                 in_=ps7b[:, :].rearrange("p (k t) -> p k t", k=8), func=AF.Copy),
                     R=[B_ps[7]], W=[B_hT])

        def load_x(tok0):
            for sub in range(4):
                T.dma("sp", "x%d" % sub, out=xt_h[:, sub * D:(sub + 1) * D],
                      in_=x_d[tok0 + sub * 128: tok0 + (sub + 1) * 128, :], W=[B_xt[sub]])

        inb = [0]

        def in_bank():
            b = inb[0] % 2
            inb[0] += 1
            return b

        def mixer_phase(s):
            for h in range(8):
                T.dma("pool", "cc", out=kT[64:72, h, :], in_=khot_d[:, :], W=[B_khot])
            T.op("pool", lambda: G.memset(vaug[:, :, :, 64:128].rearrange("p a b c -> p (a b) c"), 1.0), W=B_va)
            T.op("pool", lambda: G.memset(Msel.rearrange("p h c -> p (h c)"), 0.0), W=[B_Msel])
            T.op("pool", lambda: G.memset(kmean.rearrange("p h j -> p (h j)"), 0.0), W=[B_kmean])
            T.op("pool", lambda: G.memset(xr.rearrange("p c t -> p (c t)"), 0.0), W=B_xr)
            T.op("pool", lambda: G.memset(state, 0.0), W=[B_state])
            chunks = [(t, c) for t in range(4) for c in range(10)]
            dptr = [0]

            def issue_chunk():
                if dptr[0] >= len(chunks):
                    return
                gidx = dptr[0]
                _, c = chunks[gidx]
                slot = gidx % 3
                T.dma("pool", "winr%d" % slot, out=winr[slot], in_=w_in_v[:, :, c * 256:(c + 1) * 256], W=[B_winr[slot]])
                dptr[0] += 1

            issue_chunk()
            issue_chunk()
            load_x(s * SEQ)
            norm4_B(norm4_A(), gpre_mix)
            load_x(s * SEQ + 512)
            gidx = 0
            nxt = [None]
            for t in range(4):
                tcol = slice(t * 512, (t + 1) * 512)
                ck(1)
                for c in range(10):
                    ck(1.0 + 0.05 * c + 0.01)
                    issue_chunk()
                    slot = gidx % 3
                    W_ = winr[slot]
                    BW = B_winr[slot]
                    gidx += 1
                    if c < 4:
                        for hh in range(4):
                            h = (c % 2) * 4 + hh
                            b = in_bank()
                            for k in range(8):
                                T.op("pe", lambda: P.matmul(ps[b][0:64, :], lhsT=W_[:, k, hh * 64:(hh + 1) * 64],
                                                            rhs=hT_v[:, k, :], start=(k == 0), stop=(k == 7)),
                                     R=[BW, B_hT], W=[B_ps[b]], inc=(k == 7))
                            if c < 2:
                                T.op("act", lambda: S.activation(out=qT[0:64, h, :], in_=ps[b][0:64, :], func=AF.Copy,
                                                                 scale=0.125), R=[B_ps[b]], W=[B_qT])
                            else:
                                ks, bks = stat()
                                for jb in range(2):
                                    T.op("act", lambda: S.activation(out=kT[0:64, h, t * 512 + jb * 256: t * 512 + (jb + 1) * 256],
                                                                     in_=ps[b][0:64, jb * 256:(jb + 1) * 256], func=AF.Copy,
                                                                     accum_out=ks[0:64, jb:jb + 1]),
                                         R=[B_ps[b]], W=[B_kT[t], bks])
                                T.op("dve", lambda: V.tensor_scalar(out=kmean[0:64, h, 2 * t:2 * t + 2], in0=ks[0:64, 0:2],
                                                                    scalar1=1.0 / 256, scalar2=None, op0=ALU.mult),
                                     R=[bks], W=[B_kmean])
                    elif c < 6:
                        cp = c - 4
                        for sub in range(4):
                            b = in_bank()
                            for k in range(8):
                                T.op("pe", lambda: P.matmul(ps[b][:, 0:256], lhsT=hT_v[:, k, sub * 128:(sub + 1) * 128],
                                                            rhs=W_[:, k, :], start=(k == 0), stop=(k == 7)),
                                     R=[BW, B_hT], W=[B_ps[b]], inc=(k == 7))
                            pv4 = ps[b][:, 0:256].rearrange("p (a e c) -> p a e c", a=2, e=2)
                            kt = 4 * t + sub
                            T.op("act", lambda: S.activation(out=vaug[:, kt, 2 * cp:2 * cp + 2, 0:64], in_=pv4[:, :, 0, :],
                                                             func=AF.Copy), R=[B_ps[b]], W=[B_va[t]])
                            T.op("dve", lambda: V.tensor_copy(out=vaug[:, kt, 2 * cp:2 * cp + 2, 128:192], in_=pv4[:, :, 1, :]),
                                 R=[B_ps[b]], W=[B_va[t]])
                    else:
                        for cc in range(2):
                            ch = 2 * ((c - 6) % 2) + cc
                            b = in_bank()
                            for k in range(8):
                                T.op("pe", lambda: P.matmul(ps[b][:, :], lhsT=W_[:, k, cc * 128:(cc + 1) * 128],
                                                            rhs=hT_v[:, k, :], start=(k == 0), stop=(k == 7)),
                                     R=[BW, B_hT], W=[B_ps[b]], inc=(k == 7))
                            if c < 8:
                                T.op("dve", lambda: V.tensor_copy(out=xr[:, ch, 3:515], in_=ps[b][:, :]),
                                     R=[B_ps[b]], W=[B_xr[ch]])
                            else:
                                T.op("act", lambda: S.activation(out=gg[:, ch, :], in_=ps[b][:, :], func=AF.Gelu_apprx_tanh),
                                     R=[B_ps[b]], W=[B_gg[ch]])
                ck(2)
                for sub in range(4):
                    own = 2 * t + sub // 2
                    scol = slice(sub * 128, (sub + 1) * 128)
                    for h in range(8):
                        T.op("pe", lambda: P.matmul(ps[6][:, sub * 64 + h * 8: sub * 64 + h * 8 + 8], lhsT=qT[0:64, h, scol],
                                                    rhs=kmean[0:64, h, :], start=True, stop=True),
                             R=[B_qT, B_kmean], W=[B_ps[6]], inc=(h == 7))
                    T.op("dve", lambda: V.tensor_tensor(out=gm, in0=ps[6][:, sub * 64:(sub + 1) * 64],
                                                        in1=pm_h[:, own * 64:(own + 1) * 64], op=ALU.add),
                         R=[B_ps[6], B_const], W=[B_gm])
                    for h in range(8):
                        T.op("dve", lambda: V.max(out=top8[:, h * 8:(h + 1) * 8], in_=gm[:, h * 8:(h + 1) * 8]),
                             R=[B_gm], W=[B_top8])
                    T.op("dve", lambda: V.tensor_scalar(out=thr, in0=top8.rearrange("p (h j) -> p h j", h=8)[:, :, 3],
                                                        scalar1=-1e29, scalar2=None, op0=ALU.max), R=[B_top8], W=[B_thr])
                    T.op("dve", lambda: V.tensor_tensor(out=selb.rearrange("p (h j) -> p h j", h=8),
                                                        in0=gm.rearrange("p (h j) -> p h j", h=8),
                                                        in1=thr.unsqueeze(2).to_broadcast([128, 8, 8]), op=ALU.is_ge),
                         R=[B_gm, B_thr], W=[B_selb])
                    T.op("dve", lambda: V.tensor_scalar(out=Msel[:, :, 64:72], in0=selb.rearrange("p (h j) -> p h j", h=8),
                                                        scalar1=-NEG, scalar2=NEG, op0=ALU.mult, op1=ALU.add),
                         R=[B_selb], W=[B_Msel])
                    for h in range(8):
                        T.op("pe", lambda: P.transpose(ps7b[0:72, h * 128:(h + 1) * 128], Msel[:, h, :], ident_h[:, :]),
                             R=[B_Msel, B_const], W=[B_ps[7]], inc=(h == 7))
                    T.op("act", lambda: S.activation(out=qT[64:72, :, scol],
                                                     in_=ps7b[64:72, :].rearrange("p (h t) -> p h t", h=8), func=AF.Copy),
                         R=[B_ps[7]], W=[B_qTa])
                ck(3)
                def lru_A(ch):
                    lcol = lambda e: lc[:, ch, e:e + 1]
                    T.op("dve", lambda: V.tensor_scalar(out=ybuf, in0=xr[:, ch, 3:515], scalar1=lcol(3), scalar2=lcol(4),
                                                        op0=ALU.mult, op1=ALU.add), R=[B_xr[ch], B_const], W=[B_y])
                    for j in (2, 1, 0):
                        T.op("dve", lambda: V.scalar_tensor_tensor(out=ybuf, in0=xr[:, ch, j:j + 512], scalar=lcol(j), in1=ybuf,
                                                                   op0=ALU.mult, op1=ALU.add), R=[B_xr[ch], B_const], W=[B_y])
                    T.op("pool", lambda: G.tensor_copy(out=xr[:, ch, 0:3], in_=xr[:, ch, 512:515]), W=[B_xr[ch]])
                    T.op("act", lambda: S.activation(out=ybf, in_=ybuf, func=AF.Copy), R=[B_y], W=[B_ybf])

                def lru_B(ch):
                    lcol = lambda e: lc[:, ch, e:e + 1]
                    T.op("pe", lambda: P.matmul(ps[0][:, :], lhsT=wrbd_h[:, ch * 128:(ch + 1) * 128], rhs=ybf, start=True, stop=True),
                         R=[B_ybf, B_const], W=[B_ps[0]])
                    T.op("pe", lambda: P.matmul(ps[1][:, :], lhsT=wibd_h[:, ch * 128:(ch + 1) * 128], rhs=ybf, start=True, stop=True),
                         R=[B_ybf, B_const], W=[B_ps[1]])
                    T.op("act", lambda: S.activation(out=rb, in_=ps[0][:, :], func=AF.Sigmoid, bias=lcol(5), scale=1.0),
                         R=[B_ps[0], B_const], W=[B_rb])
                    T.op("act", lambda: S.activation(out=ib, in_=ps[1][:, :], func=AF.Sigmoid, bias=lcol(6), scale=1.0),
                         R=[B_ps[1], B_const], W=[B_ib])
                    T.op("act", lambda: S.activation(out=rb, in_=rb, func=AF.Exp, scale=neg8sp[:, ch:ch + 1]),
                         R=[B_misc], W=[B_rb])
                    T.op("dve", lambda: V.tensor_tensor(out=ub, in0=ib, in1=ybuf, op=ALU.mult), R=[B_ib, B_y], W=[B_ub])
                    T.op("dve", lambda: V.tensor_tensor(out=ib, in0=rb, in1=rb, op=ALU.mult), R=[B_rb], W=[B_ib])
                    T.op("act", lambda: S.activation(out=ib, in_=ib, func=AF.Sqrt, bias=one_ap, scale=-1.0),
                         R=[B_misc], W=[B_ib])
                    T.op("dve", lambda: V.tensor_tensor(out=ub, in0=ub, in1=ib, op=ALU.mult), R=[B_ib], W=[B_ub])
                    T.op("dve", lambda: V.tensor_tensor_scan(out=hs, data0=rb, data1=ub, initial=state[:, ch:ch + 1],
                                                             op0=ALU.mult, op1=ALU.add), R=[B_rb, B_ub, B_state], W=[B_hs])
                    T.op("dve", lambda: V.tensor_copy(out=state[:, ch:ch + 1], in_=hs[:, 511:512]), R=[B_hs], W=[B_state])
                    T.op("pool", lambda: G.tensor_tensor(out=E_v[:, 4 + ch, tcol], in0=hs, in1=gg[:, ch, :], op=ALU.mult),
                         R=[B_hs, B_gg[ch]], W=[B_E[t]])

                lru_A(0)
                ck(4)
                pend = None

                def finalize(h, b):
                    odd = h % 2
                    if odd:
                        dp, np_ = slice(0, 1), slice(64, 128)
                    else:
                        dp, np_ = slice(64, 65), slice(0, 64)
                    T.op("dve", lambda: V.reciprocal(out=rden[dp, :], in_=ps[b][dp, :]), R=[B_ps[b]], W=[B_rden])
                    if odd:
                        T.op("pe", lambda: P.matmul(ps[6][0:128, :], lhsT=onesf_h[0:1, 0:128], rhs=rden[0:1, :], start=True, stop=True),
                             R=[B_rden, B_misc], W=[B_ps[6]])
                    else:
                        T.op("pe", lambda: P.matmul(ps[6][0:64, :], lhsT=onesf_h[64:65, 0:64], rhs=rden[64:65, :], start=True, stop=True),
                             R=[B_rden, B_misc], W=[B_ps[6]])
                    T.op("act", lambda: S.activation(out=bcsb[np_, :], in_=ps[6][np_, :], func=AF.Copy), R=[B_ps[6]], W=[B_bcsb])
                    T.op("dve", lambda: V.tensor_tensor(out=E_v[np_, h // 2, tcol], in0=ps[b][np_, :], in1=bcsb[np_, :], op=ALU.mult),
                         R=[B_ps[b], B_bcsb], W=[B_E[t]])

                nkt = 4 * t + 4
                items = [(h, ki) for h in range(8) for ki in range(nkt)]

                def geom(ki):
                    r = ki - 4 * t
                    c0 = 0 if r < 0 else 128 * r
                    return r, c0

                def emit_qk(idx):
                    h, ki = items[idx]
                    r, c0 = geom(ki)
                    sb_ = 2 + (idx % 2)
                    need_bias = r >= -1
                    T.op("pe", lambda: P.matmul(ps[sb_][:, c0:512], lhsT=kT[0:72, h, ki * 128:(ki + 1) * 128],
                                                rhs=qT[0:72, h, c0:512], start=True, stop=not need_bias),
                         R=[B_kT[ki // 4], B_khot, B_qT, B_qTa], W=[B_ps[sb_]], inc=not need_bias)
                    if need_bias:
                        if r == -1:
                            bc0, bc1, bb0 = 0, 128, 128
                        elif r == 3:
                            bc0, bc1, bb0 = 384, 512, 0
                        else:
                            bc0, bc1, bb0 = 128 * r, 128 * r + 256, 0
                        T.op("pe", lambda: P.matmul(ps[sb_][:, bc0:bc1], lhsT=ident_h[:, :],
                                                    rhs=btb_h[:, h * 256 + bb0: h * 256 + bb0 + (bc1 - bc0)],
                                                    start=False, stop=True),
                             R=[B_const], W=[B_ps[sb_]])

                def emit_exp_pv(idx):
                    h, ki = items[idx]
                    r, c0 = geom(ki)
                    sb_ = 2 + (idx % 2)
                    pt_i = idx % 3
                    pvb = 4 + (h % 2)
                    pr = h // 2
                    T.op("act", lambda: S.activation(out=pT[pt_i][:, c0:512], in_=ps[sb_][:, c0:512], func=AF.Exp,
                                                     bias=c31_h[:, h:h + 1], scale=1.0),
                         R=[B_ps[sb_], B_const], W=[B_pT[pt_i]])
                    if h % 2 == 0:
                        T.op("pe", lambda: P.matmul(ps[pvb][0:65, c0:512], lhsT=vaug[:, ki, pr, 0:65], rhs=pT[pt_i][:, c0:512],
                                                    start=(ki == 0), stop=(ki == nkt - 1)),
                             R=[B_va[ki // 4], B_pT[pt_i]], W=[B_ps[pvb]], inc=(ki == nkt - 1))
                    else:
                        T.op("pe", lambda: P.matmul(ps[pvb][0:128, c0:512], lhsT=vaug[:, ki, pr, 64:192], rhs=pT[pt_i][:, c0:512],
                                                    start=(ki == 0), stop=(ki == nkt - 1)),
                             R=[B_va[ki // 4], B_pT[pt_i]], W=[B_ps[pvb]], inc=(ki == nkt - 1))

                pend = None
                emit_qk(0)
                for idx, (h, ki) in enumerate(items):
                    if idx + 1 < len(items):
                        emit_qk(idx + 1)
                    emit_exp_pv(idx)
                    if t < 3 and h == 5 and ki == 2:
                        nxt[0] = norm4_A()
                    if t < 3 and h == 6 and ki == 2:
                        norm4_B(nxt[0], gpre_mix)
                        if t + 2 <= 3:
                            load_x(s * SEQ + (t + 2) * 512)
                    if ki == 1:
                        if h % 2 == 0:
                            lru_B(h // 2)
                        elif h // 2 + 1 < 4:
                            lru_A(h // 2 + 1)
                    if ki == nkt // 2 and pend is not None:
                        finalize(*pend)
                        pend = None
                    if ki == nkt - 1:
                        pend = (h, 4 + (h % 2))
                finalize(*pend)
                ck(5)

        def ffn_phase(s):
            for q4 in range(4):
                T.dma("pool", "wout", out=wout[:, 2 * q4:2 * q4 + 2, :], in_=w_out_v[:, 2 * q4:2 * q4 + 2, :], W=[B_wout])
            T.op("pool", lambda: G.memset(halo.rearrange("p j t -> p (j t)"), 0.0), W=[B_halo])
            chunks = [(t, j) for t in range(4) for j in range(22)]
            dptr = [0]

            def issue_chunk():
                if dptr[0] >= len(chunks):
                    return
                gidx = dptr[0]
                _, j = chunks[gidx]
                slot = gidx % 3
                T.dma("pool", "wupr%d" % slot, out=wupr[slot][:, :, 0:128], in_=w_up_v[:, :, j * 128:(j + 1) * 128],
                      W=[B_wupr[slot]])
                T.dma("pool", "wupr%d" % slot, out=wupr[slot][:, :, 128:256],
                      in_=w_up_v[:, :, DFF + j * 128: DFF + (j + 1) * 128], W=[B_wupr[slot]])
                dptr[0] += 1

            issue_chunk()
            issue_chunk()
            for q11 in range(11):
                T.dma("pool", "wdown", out=wdown[:, 2 * q11:2 * q11 + 2, :], in_=w_down_v[:, 2 * q11:2 * q11 + 2, :], W=[B_wdown])
            gidx = 0
            ob = [0]

            def post_norm_residual(pa, pb, Ba, Bb, gain_ap, sub):
                ss, bss = stat()
                T.op("act", lambda: S.activation(out=junk_h[:, 0:512], in_=pa, func=AF.Square, accum_out=ss[:, 0:1]),
                     R=[Ba], W=[B_junk, bss])
                T.op("act", lambda: S.activation(out=junk_h[:, 512:1024], in_=pb, func=AF.Square, accum_out=ss[:, 1:2]),
                     R=[Bb], W=[B_junk, bss])
                rs, brs = rstd_from(ss[:, 0:1], bss, ss[:, 1:2], bss)
                T.op("dve", lambda: V.scalar_tensor_tensor(out=tmpf[:, 0:512], in0=pa, scalar=rs, in1=gain_ap[:, 0:512],
                                                           op0=ALU.mult, op1=ALU.mult), R=[Ba, brs, B_gains], W=[B_tmpf])
                T.op("dve", lambda: V.scalar_tensor_tensor(out=tmpf[:, 512:1024], in0=pb, scalar=rs, in1=gain_ap[:, 512:1024],
                                                           op0=ALU.mult, op1=ALU.mult), R=[Bb, brs, B_gains], W=[B_tmpf])
                xs = xt_h[:, sub * D:(sub + 1) * D]
                T.op("dve", lambda: V.tensor_tensor(out=xs, in0=xs, in1=tmpf, op=ALU.add), R=[B_tmpf], W=[B_xt[sub]])

            for t in range(4):
                tok0 = s * SEQ + t * 512
                load_x(tok0)
                def oproj(sub, o0):
                    ecol = slice(t * 512 + sub * 128, t * 512 + (sub + 1) * 128)
                    for half in range(2):
                        for k in range(8):
                            T.op("pe", lambda: P.matmul(ps[o0 + half][:, :], lhsT=E_v[:, k, ecol],
                                                        rhs=wout[:, k, half * 512:(half + 1) * 512], start=(k == 0), stop=(k == 7)),
                                 R=[B_E[t], B_wout], W=[B_ps[o0 + half]], inc=(k == 7))

                def chain(sub, o0):
                    post_norm_residual(ps[o0][:, :], ps[o0 + 1][:, :], B_ps[o0], B_ps[o0 + 1], gpost_mix, sub)

                oproj(0, 0)
                oproj(1, 2)
                chain(0, 0)
                oproj(2, 0)
                chain(1, 2)
                oproj(3, 2)
                norm_to_hT(0, gpre_ffn)
                chain(2, 0)
                norm_to_hT(1, gpre_ffn)
                chain(3, 2)
                norm_to_hT(2, gpre_ffn)
                norm_to_hT(3, gpre_ffn)
                ck(11)
                for j in range(22):
                    issue_chunk()
                    slot = gidx % 3
                    gidx += 1
                    W_ = wupr[slot]
                    BW = B_wupr[slot]
                    pu = 4 + 2 * (j % 2)
                    pg = 5 + 2 * (j % 2)
                    bi = j % 2
                    for k in range(8):
                        T.op("pe", lambda: P.matmul(ps[pu][:, :], lhsT=W_[:, k, 0:128], rhs=hT_v[:, k, :], start=(k == 0), stop=(k == 7)),
                             R=[BW, B_hT], W=[B_ps[pu]], inc=(k == 7))
                    for k in range(8):
                        T.op("pe", lambda: P.matmul(ps[pg][:, :], lhsT=W_[:, k, 128:256], rhs=hT_v[:, k, :], start=(k == 0), stop=(k == 7)),
                             R=[BW, B_hT], W=[B_ps[pg]], inc=(k == 7))
                    T.op("pool", lambda: G.tensor_copy(out=xub[bi][:, 0:2], in_=halo[:, j, :]), R=[B_halo], W=[B_xub[bi]])
                    T.op("pool", lambda: G.tensor_copy(out=xgb[bi][:, 0:2], in_=halo[:, 22 + j, :]), R=[B_halo], W=[B_xgb[bi]])
                    T.op("act", lambda: S.activation(out=xgb[bi][:, 2:514], in_=ps[pg][:, :], func=AF.Copy), R=[B_ps[pg]], W=[B_xgb[bi]])
                    T.op("dve", lambda: V.tensor_copy(out=xub[bi][:, 2:514], in_=ps[pu][:, :]), R=[B_ps[pu]], W=[B_xub[bi]])
                    fc = ffncols_h[:, :].rearrange("p (j e) -> p j e", j=44)
                    def conv_first(src, bsrc, dst, bdst, jj):
                        T.op("dve", lambda: V.tensor_scalar(out=dst, in0=src[:, 2:514], scalar1=fc[:, jj, 2:3], scalar2=fc[:, jj, 3:4],
                                                            op0=ALU.mult, op1=ALU.add), R=[bsrc, B_const], W=[bdst])

                    def conv_tap(src, bsrc, dst, bdst, jj, tap):
                        T.op("dve", lambda: V.scalar_tensor_tensor(out=dst, in0=src[:, tap:tap + 512], scalar=fc[:, jj, tap:tap + 1],
                                                                   in1=dst, op0=ALU.mult, op1=ALU.add), R=[bsrc, B_const], W=[bdst])

                    ga = (xgb[bi], B_xgb[bi], cg, B_cg, 22 + j)
                    ua = (xub[bi], B_xub[bi], cu, B_cu, j)
                    T.op("act", lambda: S.activation(out=cg, in_=ps[pg][:, :], func=AF.Identity, bias=fc[:, 22 + j, 3:4],
                                                     scale=fc[:, 22 + j, 2:3]), R=[B_ps[pg], B_const], W=[B_cg])
                    T.op("act", lambda: S.activation(out=cu, in_=ps[pu][:, :], func=AF.Identity, bias=fc[:, j, 3:4],
                                                     scale=fc[:, j, 2:3]), R=[B_ps[pu], B_const], W=[B_cu])
                    conv_tap(*ga, 1)
                    conv_tap(*ua, 1)
                    conv_tap(*ga, 0)
                    T.op("act", lambda: S.activation(out=gl, in_=cg, func=AF.Gelu_apprx_tanh), R=[B_cg], W=[B_gl])
                    conv_tap(*ua, 0)
                    T.op("pool", lambda: G.tensor_copy(out=halo[:, j, :], in_=xub[bi][:, 512:514]), R=[B_xub[bi]], W=[B_halo])
                    T.op("pool", lambda: G.tensor_copy(out=halo[:, 22 + j, :], in_=xgb[bi][:, 512:514]), R=[B_xgb[bi]], W=[B_halo])
                    T.op("dve", lambda: V.tensor_tensor(out=actT[:, j, :], in0=cu, in1=gl, op=ALU.mult), R=[B_cu, B_gl], W=[B_actT])
                ck(12)
                for sub in range(4):
                    o0 = 2 * (ob[0] % 2)
                    ob[0] += 1
                    for half in range(2):
                        for j in range(22):
                            T.op("pe", lambda: P.matmul(ps[o0 + half][:, :], lhsT=actT[:, j, sub * 128:(sub + 1) * 128],
                                                        rhs=wdown[:, j, half * 512:(half + 1) * 512], start=(j == 0), stop=(j == 21)),
                                 R=[B_actT, B_wdown], W=[B_ps[o0 + half]], inc=(j == 21))
                    post_norm_residual(ps[o0][:, :], ps[o0 + 1][:, :], B_ps[o0], B_ps[o0 + 1], gpost_ffn, sub)
                    T.dma("sp", "y%d" % sub, out=y_d[tok0 + sub * 128: tok0 + (sub + 1) * 128, :],
                          in_=xt_h[:, sub * D:(sub + 1) * D], R=[B_xt[sub]])

        try:
            for s in range(2):
                T.barrier(allbufs)
                ck(0)
                mixer_phase(s)
                T.barrier(allbufs)
                ck(10)
                ffn_phase(s)
                ck(20)
        except _Stop:
            pass
        T.barrier(allbufs)
    return nc


def _bucket(d):
    n = np.maximum(d, 0)
    nf = np.maximum(n, 1).astype(np.float32)
    large = 16 + (np.log(nf / np.float32(16)) / np.float32(math.log(128 / 16)) * np.float32(16)).astype(np.int32)
    large = np.minimum(large, 31)
    return np.where(n < 16, n, large)


_NC_CACHE = {}


def kernel(x, g_pre_mix, w_in, rel_bias, w_conv_lru, b_conv_lru, w_r, b_r, w_i, b_i, lam,
           w_out, g_post_mix, g_pre_ffn, w_up, w_conv_ffn, b_conv_ffn, w_down, g_post_ffn):
    f32 = lambda a: np.ascontiguousarray(np.asarray(a, dtype=np.float32))
    x = f32(x)
    rel_bias = f32(rel_bias)
    gains = np.stack([f32(g_pre_mix)[0], f32(g_post_mix)[0], f32(g_pre_ffn)[0], f32(g_post_ffn)[0]], 0)
    cols = np.concatenate([f32(w_conv_lru)[0], f32(b_conv_lru), f32(b_r), f32(b_i), f32(lam)], 0)
    lrucols = np.ascontiguousarray(cols.reshape(8, 4, 128).transpose(2, 1, 0)).reshape(128, 32)
    wr = f32(w_r)[0]
    wi = f32(w_i)[0]
    wrbd = np.zeros((128, 4, 128), np.float32)
    wibd = np.zeros((128, 4, 128), np.float32)
    for c in range(4):
        for e in range(2):
            wrbd[e * 64:(e + 1) * 64, c, e * 64:(e + 1) * 64] = wr[2 * c + e]
            wibd[e * 64:(e + 1) * 64, c, e * 64:(e + 1) * 64] = wi[2 * c + e]
    fcols = np.concatenate([f32(w_conv_ffn)[0], f32(b_conv_ffn)], 0)
    ffncols = np.ascontiguousarray(fcols.reshape(4, 44, 128).transpose(2, 1, 0)).reshape(128, 176)
    kk = np.arange(128)[:, None]
    cc = np.arange(256)[None, :]
    dist = cc - kk
    gath = rel_bias[_bucket(dist)]
    btraw = np.where((dist >= 0)[:, :, None], gath, np.float32(NEG)).astype(np.float32)
    btraw = np.ascontiguousarray(btraw.transpose(0, 2, 1)).reshape(128, 2048)
    c31 = np.ascontiguousarray(rel_bias[31:32, :])
    khot = np.zeros((8, SEQ), np.float32)
    for j in range(8):
        khot[j, j * 256:(j + 1) * 256] = 1.0
    ident = np.eye(128, dtype=np.float32)
    pm = np.zeros((8, 8, 8), np.float32)
    for own in range(8):
        for j in range(8):
            pm[own, :, j] = 0.0 if j < own else (1e30 if j == own else -2e30)
    pm = pm.reshape(1, 512)
    shared = {
        "w_in": f32(w_in)[0], "w_out": f32(w_out)[0], "w_up": f32(w_up)[0], "w_down": f32(w_down)[0],
        "gains": gains, "lrucols": lrucols, "wrbd": wrbd.reshape(128, 512), "wibd": wibd.reshape(128, 512),
        "ffncols": ffncols, "btraw": btraw, "c31": c31, "khot": khot, "ident": ident, "pm": pm,
    }
    in_maps = []
    for c in range(NCORES):
        m = dict(shared)
        m["x"] = x[2 * c:2 * c + 2].reshape(2 * SEQ, D)
        in_maps.append(m)
    if "nc" not in _NC_CACHE:
        _NC_CACHE["nc"] = build_nc()
    nc = _NC_CACHE["nc"]
    res = run_bass_kernel_spmd(nc, in_maps, core_ids=list(range(NCORES)))
    out = np.stack([np.asarray(r["y"], dtype=np.float32).reshape(2, SEQ, D) for r in res.results], 0)
    return out.reshape(16, SEQ, D)
```
